# Optimizing a Trainium2 kernel written in Bass

```python
import functools
import jax, jax.numpy as jnp
from jax import lax
import numpy as np

D_MODEL = 2048
BATCH = 2
SEQ = 4096
DEPTH = 1
DEC_BATCH = 8
DEC_SEQ = 4
PAST_LEN = 16384
PAGE_SIZE = 128

N_ATTN_HEADS = 8
ATTN_HEAD_DIM = 128
ATTN_WIDTH = N_ATTN_HEADS * ATTN_HEAD_DIM
MOBA_BLOCK = 256
MOBA_TOPK = 3
ATTN_Q_BLOCK = 64
N_REC_HEADS = 8
REC_KEY_DIM = 128
REC_VAL_DIM = 128
REC_KEY_WIDTH = N_REC_HEADS * REC_KEY_DIM
REC_VAL_WIDTH = N_REC_HEADS * REC_VAL_DIM
REC_CHUNK = 64
OFF_QA = 0
OFF_KA = OFF_QA + ATTN_WIDTH
OFF_VA = OFF_KA + ATTN_WIDTH
OFF_QR = OFF_VA + ATTN_WIDTH
OFF_FR = OFF_QR + REC_KEY_WIDTH
OFF_IR = OFF_FR + REC_KEY_WIDTH
OFF_OG = OFF_IR + REC_VAL_WIDTH
OFF_GA = OFF_OG + REC_VAL_WIDTH
OFF_GB = OFF_GA + D_MODEL
IN_WIDTH = OFF_GB + D_MODEL
N_GROUPS = 4
EXPERTS_PER_GROUP = 8
N_EXPERTS = N_GROUPS * EXPERTS_PER_GROUP
TOP_K = 2
D_EXPERT = 512
MOE_BLOCK = 128
LN_EPS = 1e-5
RMS_EPS = 1e-6
DEEPNORM_ALPHA = (2 * DEPTH) ** 0.25
DEEPNORM_BETA = (8 * DEPTH) ** -0.25

kernel_name = 'moba_hgrn2_hmoe_deepnorm_step'


def layer_norm(x, g, b):
    xf = x.astype(jnp.float32)
    mu = jnp.mean(xf, -1, keepdims=True)
    xc = xf - mu
    var = jnp.mean(xc * xc, -1, keepdims=True)
    return (xc * lax.rsqrt(var + LN_EPS) * g.astype(jnp.float32) + b.astype(jnp.float32)).astype(x.dtype)


def alibi_slopes():
    return 2.0 ** (-8.0 * jnp.arange(1, N_ATTN_HEADS + 1, dtype=jnp.float32) / N_ATTN_HEADS)


def moba_blocks(k, v):
    b, l, h, hd = k.shape
    nb = -(-l // MOBA_BLOCK)
    pad = nb * MOBA_BLOCK - l
    def blk(t):
        t = jnp.pad(t, ((0, 0), (0, pad), (0, 0), (0, 0)))
        return t.reshape(b, nb, MOBA_BLOCK, h, hd).transpose(0, 3, 1, 2, 4)
    kb, vb = blk(k), blk(v)
    k_mean = jnp.mean(kb.astype(jnp.float32), axis=3)
    return kb, vb, k_mean


def moba_core(q, q_pos, kb, vb, k_mean):
    b, h, nq, hd = q.shape
    nb = kb.shape[2]
    own = q_pos // MOBA_BLOCK
    gate = jnp.einsum('bhqd,bhnd->bhqn', q, k_mean)
    fully_past = jnp.arange(nb)[None, :] < own[:, None]
    gate = jnp.where(fully_past[None, None], gate, -jnp.inf)
    if nb < MOBA_TOPK:
        gate = jnp.pad(gate, ((0, 0), (0, 0), (0, 0), (0, MOBA_TOPK - nb)), constant_values=-jnp.inf)
    _, top_i = lax.top_k(gate, MOBA_TOPK)
    top_i = jnp.minimum(top_i, nb - 1)
    sel = jnp.concatenate([top_i, jnp.broadcast_to(own[None, None, :, None], (b, h, nq, 1))], -1)
    valid_blk = jnp.concatenate([jnp.arange(MOBA_TOPK)[None, :] < own[:, None],
                                 jnp.ones((nq, 1), bool)], -1)
    bi = jnp.arange(b)[:, None, None, None]
    hi = jnp.arange(h)[None, :, None, None]
    ks = kb[bi, hi, sel]
    vs = vb[bi, hi, sel]
    s = jnp.einsum('bhqd,bhqjkd->bhqjk', q.astype(ks.dtype), ks).astype(jnp.float32)
    key_pos = sel[..., None] * MOBA_BLOCK + jnp.arange(MOBA_BLOCK)
    dist = q_pos[None, None, :, None, None] - key_pos
    mask = valid_blk[None, None, :, :, None] & (dist >= 0)
    s = jnp.where(mask, s - alibi_slopes()[None, :, None, None, None] * dist.astype(jnp.float32), -jnp.inf)
    nj = sel.shape[-1]
    p = jax.nn.softmax(s.reshape(b, h, nq, nj * MOBA_BLOCK), axis=-1).reshape(s.shape)
    return jnp.einsum('bhqjk,bhqjkd->bhqd', p.astype(vs.dtype), vs).astype(jnp.float32)


def moba_prompt(q, k, v):
    b, s_len, h, hd = q.shape
    kb, vb, km = moba_blocks(k, v)
    qc = min(ATTN_Q_BLOCK, s_len)
    nc = s_len // qc
    qt = (q.astype(jnp.float32) * hd ** -0.5).reshape(b, nc, qc, h, hd).transpose(1, 0, 3, 2, 4)
    pos = jnp.arange(s_len, dtype=jnp.int32).reshape(nc, qc)
    o = lax.map(lambda a: moba_core(a[0], a[1], kb, vb, km), (qt, pos))
    return o.transpose(1, 0, 3, 2, 4).reshape(b, s_len, h, hd).astype(q.dtype)


def moba_sample(q, k, v, cache_k_l, cache_v_l, page_table):
    db, t, h, hd = q.shape
    past_k = cache_k_l[page_table].reshape(db, -1, h, hd)
    past_v = cache_v_l[page_table].reshape(db, -1, h, hd)
    past_len = past_k.shape[1]
    k_all = jnp.concatenate([past_k.astype(k.dtype), k], 1)
    v_all = jnp.concatenate([past_v.astype(v.dtype), v], 1)
    kb, vb, km = moba_blocks(k_all, v_all)
    pos = past_len + jnp.arange(t, dtype=jnp.int32)
    qt = (q.astype(jnp.float32) * hd ** -0.5).transpose(0, 2, 1, 3)
    o = moba_core(qt, pos, kb, vb, km)
    return o.transpose(0, 2, 1, 3).astype(q.dtype)


def hgrn2_recurrence(q, k, v, g, s0):
    b, t = q.shape[:2]
    c = min(REC_CHUNK, t)
    nc = -(-t // c)
    pad = nc * c - t
    def chunks(x):
        x = jnp.pad(x, ((0, 0), (0, pad), (0, 0), (0, 0)))
        return x.reshape(b, nc, c, x.shape[2], x.shape[3]).transpose(1, 0, 3, 2, 4)
    causal = jnp.tril(jnp.ones((c, c), bool))
    def step(s, inp):
        qc, kc, vc, gc = inp
        G = jnp.cumsum(gc, axis=2)
        o_inter = jnp.einsum('bhtk,bhkv->bhtv', qc * jnp.exp(G), s)
        diff = jnp.where(causal[None, None, :, :, None], G[:, :, :, None, :] - G[:, :, None, :, :], -jnp.inf)
        a = jnp.einsum('bhtk,bhsk,bhtsk->bhts', qc, kc, jnp.exp(diff))
        o_intra = jnp.einsum('bhts,bhsv->bhtv', a, vc)
        g_last = G[:, :, -1:, :]
        s_new = jnp.exp(g_last[:, :, 0, :, None]) * s + jnp.einsum('bhsk,bhsv->bhkv', kc * jnp.exp(g_last - G), vc)
        return s_new, o_inter + o_intra
    s_fin, o = lax.scan(step, s0, (chunks(q), chunks(k), chunks(v), chunks(g)))
    o = o.transpose(1, 0, 3, 2, 4).reshape(b, nc * c, q.shape[2], v.shape[3])[:, :t]
    return o, s_fin


def token_mixers(x, attend, rec_state0, w_in, lb, rec_norm_g, w_pa, w_pb, w_out):
    b, t, _ = x.shape
    f32 = jnp.float32
    proj = x @ w_in
    def heads(lo, hi, n, dh):
        return proj[..., lo:hi].reshape(b, t, n, dh)
    q = heads(OFF_QA, OFF_KA, N_ATTN_HEADS, ATTN_HEAD_DIM)
    k = heads(OFF_KA, OFF_VA, N_ATTN_HEADS, ATTN_HEAD_DIM)
    v = heads(OFF_VA, OFF_QR, N_ATTN_HEADS, ATTN_HEAD_DIM)
    o_attn = attend(q, k, v).reshape(b, t, ATTN_WIDTH)
    q_r = jax.nn.silu(heads(OFF_QR, OFF_FR, N_REC_HEADS, REC_KEY_DIM).astype(f32))
    forget = lb + (1.0 - lb) * jax.nn.sigmoid(proj[..., OFF_FR:OFF_IR].astype(f32))
    forget = forget.reshape(b, t, N_REC_HEADS, REC_KEY_DIM)
    i_r = heads(OFF_IR, OFF_OG, N_REC_HEADS, REC_VAL_DIM).astype(f32)
    o_rec, rec_state = hgrn2_recurrence(q_r, 1.0 - forget, i_r, jnp.log(forget), rec_state0.astype(f32))
    o_rec = o_rec * lax.rsqrt(jnp.mean(jnp.square(o_rec), -1, keepdims=True) + RMS_EPS) \
        * rec_norm_g.astype(f32).reshape(N_REC_HEADS, REC_VAL_DIM)
    o_rec = (o_rec * jax.nn.sigmoid(heads(OFF_OG, OFF_GA, N_REC_HEADS, REC_VAL_DIM).astype(f32)))
    o_rec = o_rec.reshape(b, t, REC_VAL_WIDTH).astype(x.dtype)
    gate_a = jax.nn.sigmoid(proj[..., OFF_GA:OFF_GB])
    gate_b = jax.nn.sigmoid(proj[..., OFF_GB:IN_WIDTH])
    merged = gate_a * (o_attn @ w_pa) + gate_b * (o_rec @ w_pb)
    return merged @ w_out, k, v, rec_state.astype(rec_state0.dtype)


def hier_moe(xt, w_group, b_group, w_router, b_router, w1, w3, w2):
    n, d = xt.shape
    f32 = jnp.float32
    g_logits = (xt @ w_group).astype(f32) + b_group.astype(f32)
    g_idx = jnp.argmax(g_logits, -1)
    g_w = jnp.take_along_axis(jax.nn.softmax(g_logits, -1), g_idx[:, None], 1)
    e_logits = jnp.einsum('nd,gde->nge', xt, w_router).astype(f32) + b_router.astype(f32)
    e_logits = jnp.take_along_axis(e_logits, g_idx[:, None, None], 1)[:, 0]
    top_v, top_i = lax.top_k(e_logits, TOP_K)
    gate = g_w * jax.nn.softmax(top_v, -1)
    expert = g_idx[:, None].astype(jnp.int32) * EXPERTS_PER_GROUP + top_i.astype(jnp.int32)
    flat_e = expert.reshape(-1)
    flat_w = gate.reshape(-1)
    nk = flat_e.shape[0]
    order = jnp.argsort(flat_e)
    e_sorted = flat_e[order]
    counts = jnp.bincount(flat_e, length=N_EXPERTS)
    padded = (counts + MOE_BLOCK - 1) // MOE_BLOCK * MOE_BLOCK
    start = jnp.cumsum(counts) - counts
    pstart = jnp.cumsum(padded) - padded
    dest = pstart[e_sorted] + jnp.arange(nk) - start[e_sorted]
    n_blk = -(-nk // MOE_BLOCK) + N_EXPERTS
    rows = n_blk * MOE_BLOCK
    row_tok = jnp.zeros((rows,), jnp.int32).at[dest].set((order // TOP_K).astype(jnp.int32))
    row_w = jnp.zeros((rows,), f32).at[dest].set(flat_w[order])
    blk_start = jnp.arange(n_blk) * MOE_BLOCK
    blk_expert = jnp.minimum(jnp.sum((pstart + padded)[None, :] <= blk_start[:, None], axis=1), N_EXPERTS - 1)
    xb = xt[row_tok].reshape(n_blk, MOE_BLOCK, d)
    def expert_block(a):
        xblk, e = a
        hid = jax.nn.silu(xblk @ w1[e]) * (xblk @ w3[e])
        return hid @ w2[e]
    yb = lax.map(expert_block, (xb, blk_expert)).reshape(rows, d)
    return jax.ops.segment_sum(yb * row_w[:, None].astype(yb.dtype), row_tok, num_segments=n)


def trunk_layer(x, attend, rec_state0, w_in, lb, rec_norm_g, w_pa, w_pb, w_out, ln1_g, ln1_b,
                w_group, b_group, w_router, b_router, w1, w3, w2, ln2_g, ln2_b):
    h, k, v, rec_state = token_mixers(x, attend, rec_state0, w_in, lb, rec_norm_g, w_pa, w_pb, w_out)
    x1 = layer_norm(DEEPNORM_ALPHA * x + h, ln1_g, ln1_b)
    m = hier_moe(x1.reshape(-1, x1.shape[-1]), w_group, b_group, w_router, b_router, w1, w3, w2).reshape(x1.shape)
    y = layer_norm(DEEPNORM_ALPHA * x1 + m, ln2_g, ln2_b)
    return y, k, v, rec_state


def setup_inputs(seed: int = 0) -> dict:
    key = jax.random.key(seed)
    ks = jax.random.split(key, 24)
    f32 = jnp.float32
    n_pages = PAST_LEN // PAGE_SIZE
    n_pool = (DEC_BATCH * n_pages * 5) // 4
    def nrm(k, shape, scale):
        return scale * jax.random.normal(k, shape, f32)
    beta = DEEPNORM_BETA
    col_scale = jnp.ones((IN_WIDTH,), f32).at[OFF_VA:OFF_QR].set(beta).at[OFF_IR:OFF_OG].set(beta)
    return {
        'x_prompt': nrm(ks[0], (BATCH, SEQ, D_MODEL), 1.0),
        'x_sample': nrm(ks[1], (DEC_BATCH, DEC_SEQ, D_MODEL), 1.0),
        'cache_k': nrm(ks[2], (DEPTH, n_pool, PAGE_SIZE, N_ATTN_HEADS, ATTN_HEAD_DIM), 1.0),
        'cache_v': nrm(ks[3], (DEPTH, n_pool, PAGE_SIZE, N_ATTN_HEADS, ATTN_HEAD_DIM), beta),
        'state_rec': nrm(ks[4], (DEPTH, DEC_BATCH, N_REC_HEADS, REC_KEY_DIM, REC_VAL_DIM), 0.5),
        'page_table': jax.random.permutation(ks[5], n_pool)[:DEC_BATCH * n_pages].reshape(DEC_BATCH, n_pages).astype(jnp.int32),
        'w_in': nrm(ks[6], (DEPTH, D_MODEL, IN_WIDTH), D_MODEL ** -0.5) * col_scale,
        'lb_logits': nrm(ks[7], (DEPTH + 1, REC_KEY_WIDTH), 0.5),
        'rec_norm_g': 1.0 + nrm(ks[8], (DEPTH, REC_VAL_WIDTH), 0.02),
        'w_pa': nrm(ks[9], (DEPTH, ATTN_WIDTH, D_MODEL), beta * ATTN_WIDTH ** -0.5),
        'w_pb': nrm(ks[10], (DEPTH, REC_VAL_WIDTH, D_MODEL), beta * REC_VAL_WIDTH ** -0.5),
        'w_out': nrm(ks[11], (DEPTH, D_MODEL, D_MODEL), beta * D_MODEL ** -0.5),
        'ln1_g': 1.0 + nrm(ks[12], (DEPTH, D_MODEL), 0.02),
        'ln1_b': nrm(ks[13], (DEPTH, D_MODEL), 0.02),
        'w_group': nrm(ks[14], (DEPTH, D_MODEL, N_GROUPS), D_MODEL ** -0.5),
        'b_group': nrm(ks[15], (DEPTH, N_GROUPS), 0.01),
        'w_router': nrm(ks[16], (DEPTH, N_GROUPS, D_MODEL, EXPERTS_PER_GROUP), D_MODEL ** -0.5),
        'b_router': nrm(ks[17], (DEPTH, N_GROUPS, EXPERTS_PER_GROUP), 0.01),
        'w1': nrm(ks[18], (DEPTH, N_EXPERTS, D_MODEL, D_EXPERT), beta * D_MODEL ** -0.5),
        'w3': nrm(ks[19], (DEPTH, N_EXPERTS, D_MODEL, D_EXPERT), beta * D_MODEL ** -0.5),
        'w2': nrm(ks[20], (DEPTH, N_EXPERTS, D_EXPERT, D_MODEL), beta * D_EXPERT ** -0.5),
        'ln2_g': 1.0 + nrm(ks[21], (DEPTH, D_MODEL), 0.02),
        'ln2_b': nrm(ks[22], (DEPTH, D_MODEL), 0.02),
    }


def reference(x_prompt, x_sample, cache_k, cache_v, state_rec, page_table, w_in, lb_logits, rec_norm_g,
              w_pa, w_pb, w_out, ln1_g, ln1_b, w_group, b_group, w_router, b_router, w1, w3, w2, ln2_g, ln2_b):
    lower_bounds = jnp.cumsum(jax.nn.softmax(lb_logits.astype(jnp.float32), axis=0), axis=0)
    b = x_prompt.shape[0]
    xp, xs = x_prompt, x_sample
    kp_l, vp_l, sp_l, ks_l, vs_l, ss_l = [], [], [], [], [], []
    for l in range(DEPTH):
        lw = (w_in[l], lower_bounds[l], rec_norm_g[l], w_pa[l], w_pb[l], w_out[l], ln1_g[l], ln1_b[l],
              w_group[l], b_group[l], w_router[l], b_router[l], w1[l], w3[l], w2[l], ln2_g[l], ln2_b[l])
        rec0 = jnp.zeros((b,) + state_rec.shape[2:], state_rec.dtype)
        xp, kp, vp, sp = trunk_layer(xp, moba_prompt, rec0, *lw)
        attend_s = functools.partial(moba_sample, cache_k_l=cache_k[l], cache_v_l=cache_v[l], page_table=page_table)
        xs, ksm, vsm, ssm = trunk_layer(xs, attend_s, state_rec[l], *lw)
        kp_l.append(kp); vp_l.append(vp); sp_l.append(sp)
        ks_l.append(ksm); vs_l.append(vsm); ss_l.append(ssm)
    return (xp, xs, jnp.stack(kp_l), jnp.stack(vp_l), jnp.stack(sp_l), jnp.stack(ks_l), jnp.stack(vs_l), jnp.stack(ss_l))
```

```python
import numpy as np
from contextlib import ExitStack
import concourse.bass as bass
import concourse.mybir as mybir
from concourse.bass_utils import run_bass_kernel_spmd

F32 = mybir.dt.float32
BF16 = mybir.dt.bfloat16
I32 = mybir.dt.int32
U32 = mybir.dt.uint32
AF = mybir.ActivationFunctionType
ALU = mybir.AluOpType
AX = mybir.AxisListType

D = 2048
NPRE = 3072
NOWN = 1024
NS = 4
NTOK = NOWN + NS
NT_PRE = NPRE // 128
NT_OWN = NOWN // 128
INW = 11264
OFF_QA, OFF_KA, OFF_VA, OFF_QR, OFF_FR, OFF_IR, OFF_OG, OFF_GA, OFF_GB = (
    0, 1024, 2048, 3072, 4096, 5120, 6144, 7168, 9216)
NEG = -1.0e30
ALPHA = 2.0 ** 0.25
LN_EPS = 1e-5
RMS_EPS = 1e-6
NDS = 24


class Buf:
    __slots__ = ("w", "r")

    def __init__(self):
        self.w = None
        self.r = []


class Tile:
    def __init__(self, t):
        self.t = t
        self.b = Buf()
        self.subs = {}

    def __getitem__(self, k):
        return self.t[k]

    def sub(self, key):
        if key not in self.subs:
            self.subs[key] = Buf()
        return self.subs[key]


def _bufs(xs):
    out = []
    for x in xs:
        if isinstance(x, Buf):
            out.append(x)
        else:
            out.append(x.b)
    return out


class KB:
    def __init__(self, nc, es):
        self.nc = nc
        self.es = es
        self.E = {"pe": nc.tensor, "act": nc.scalar, "dve": nc.vector, "pool": nc.gpsimd, "sp": nc.sync}
        self.psem = {e: es.enter_context(nc.semaphore("p_" + e)) for e in self.E}
        self.pcnt = {e: 0 for e in self.E}
        self.seen = {e: {} for e in self.E}
        self.dsem = [es.enter_context(nc.semaphore("d%d" % i)) for i in range(NDS)]
        self.dcnt = [0] * NDS
        self.dnext = {"sw": 0, "hw": NDS // 2}
        self.nm = 0
        self.out_toks = []

    def sb(self, shape, dt, name=None):
        self.nm += 1
        t = self.es.enter_context(self.nc.sbuf_tensor(name or ("t%d" % self.nm), list(shape), dt))
        return Tile(t)

    def psum(self, shape, dt, name=None):
        self.nm += 1
        t = self.es.enter_context(self.nc.psum_tensor(name or ("p%d" % self.nm), list(shape), dt))
        return Tile(t)

    def _slot(self, kind):
        h = NDS // 2
        s = self.dnext[kind]
        base = 0 if kind == "sw" else h
        self.dnext[kind] = base + (s - base + 1) % h
        return s

    def _sem(self, key):
        return self.psem[key[1]] if key[0] == "p" else self.dsem[key[1]]

    def _wait(self, e, deps):
        best = {}
        for key, val in deps:
            if key == ("p", "pe") and e == "pe":
                continue
            if val > best.get(key, 0):
                best[key] = val
        for key, val in best.items():
            if self.seen[e].get(key, 0) >= val:
                continue
            self.E[e].wait_ge(self._sem(key), val)
            self.seen[e][key] = val

    def _deps(self, r, w):
        deps = []
        for b in r:
            if b.w is not None:
                deps.append(b.w)
        for b in w:
            if b.w is not None:
                deps.append(b.w)
            deps.extend(b.r)
        return deps

    def _commit(self, tok, r, w):
        for b in r:
            b.r.append(tok)
        for b in w:
            b.w = tok
            b.r = []

    def op(self, e, fn, r=(), w=()):
        r = _bufs(r)
        w = _bufs(w)
        self._wait(e, self._deps(r, w))
        ins = fn()
        self.pcnt[e] += 1
        ins.then_inc(self.psem[e], 1)
        tok = (("p", e), self.pcnt[e])
        self._commit(tok, r, w)
        return tok

    def dma(self, q, out, in_, r=(), w=(), is_out=False):
        r = _bufs(r)
        w = _bufs(w)
        self._wait(q, self._deps(r, w))
        s = self._slot("sw" if q == "pool" else "hw")
        if self.dcnt[s] > 0:
            self._wait(q, [(("d", s), self.dcnt[s])])
        ins = self.E[q].dma_start(out=out, in_=in_)
        self.dcnt[s] += 16
        ins.then_inc(self.dsem[s], 16)
        tok = (("d", s), self.dcnt[s])
        self._commit(tok, r, w)
        if is_out:
            self.out_toks.append(tok)
        return tok

    def gather(self, out, in_, idx_ap, r=(), w=()):
        q = "pool"
        r = _bufs(r)
        w = _bufs(w)
        self._wait(q, self._deps(r, w))
        s = self._slot("sw")
        if self.dcnt[s] > 0:
            self._wait(q, [(("d", s), self.dcnt[s])])
        ins = self.nc.gpsimd.indirect_dma_start(
            out=out, out_offset=None, in_=in_,
            in_offset=bass.IndirectOffsetOnAxis(ap=idx_ap, axis=0))
        self.dcnt[s] += 16
        ins.then_inc(self.dsem[s], 16)
        tok = (("d", s), self.dcnt[s])
        self._commit(tok, r, w)
        return tok

    def barrier(self):
        toks = [(("p", e), self.pcnt[e]) for e in self.E if self.pcnt[e] > 0]
        toks += [(("d", s), self.dcnt[s]) for s in range(NDS) if self.dcnt[s] > 0]
        for e in self.E:
            self._wait(e, [t for t in toks if t[0] != ("p", e)])

    def finish(self):
        self._wait("sp", self.out_toks)
        self._wait("sp", [(("p", e), self.pcnt[e]) for e in self.E if e != "sp" and self.pcnt[e] > 0])


def _host_consts(qt):
    c = {}
    idn = np.eye(128, dtype=np.float32)
    s = np.arange(128)[:, None]
    t = np.arange(128)[None, :]
    same = (s // 64) == (t // 64)
    c["c_id"] = idn
    c["c_mincl"] = (same & (s <= t)).astype(np.float32)
    c["c_mstrict"] = (same & (s > t)).astype(np.float32)
    c["c_msuf"] = (s > t).astype(np.float32)
    c["c_ones"] = np.ones((128, 128), np.float32)
    cind = np.zeros((128, 2), np.float32)
    cind[:64, 0] = 1
    cind[64:, 1] = 1
    c["c_cind"] = cind
    c["c_iota"] = np.arange(128, dtype=np.float32).reshape(128, 1)
    slopes = 2.0 ** (-np.arange(1, 9, dtype=np.float64))
    q = np.arange(512)[None, None, :]
    tt = np.arange(128)[:, None, None]
    jj = np.arange(4)[None, :, None]
    c["c_caus"] = np.where(q >= 128 * jj + tt, 0.0, NEG).astype(np.float32).reshape(128, 2048)
    ee = np.zeros((18, 16, 128), np.float32)
    for n in range(16):
        ee[n, n, :] = 1.0
    ee[16:18] = 1.0
    c["c_EE"] = ee.reshape(18, 2048)
    qq = np.arange(512)
    qal = np.zeros((2, 8, 512), np.float64)
    for h in range(8):
        qal[0, h] = -slopes[h] * 128 * (qq // 128)
        qal[1, h] = -slopes[h] * (qq % 128)
    c["c_qal"] = qal.reshape(2, 4096).astype(np.float32)
    alb = np.zeros((128, 8, 32), np.float64)
    for h in range(8):
        for mi in range(32):
            alb[:, h, mi] = slopes[h] * (128 * (mi - 28) + np.arange(128))
    c["c_alb"] = alb.reshape(128, 256).astype(np.float32)
    gt = np.zeros((8, 16), np.float32)
    ow = np.zeros((8, 16), np.float32)
    for i in range(8):
        n_own = 12 + i // 2
        for n in range(16):
            ok = (n < n_own) and (n >= 12 - 4 * qt)
            gt[i, n] = 0.0 if ok else NEG
        ow[i, n_own] = 1.0
    c["c_gtab"] = np.broadcast_to(gt.reshape(1, 128), (128, 128)).copy()
    c["c_own"] = np.broadcast_to(ow.reshape(1, 128), (128, 128)).copy()
    bs = np.zeros((128, 8, 2, 64), np.float64)
    for h in range(8):
        for a in range(2):
            for n in range(64):
                bs[:, h, a, n] = slopes[h] * (128 * (2 * n + a) + np.arange(128) - 16384)
    c["c_bias_s"] = bs.reshape(128, 1024).astype(np.float32)
    cs = np.zeros((4, 8, 4), np.float64)
    for tk in range(4):
        for h in range(8):
            for qi in range(4):
                cs[tk, h, qi] = slopes[h] * tk if tk <= qi else NEG
    c["c_caus_s"] = cs.reshape(4, 32).astype(np.float32)
    oq = np.zeros((4, 4, 128), np.float32)
    for qi in range(4):
        oq[qi, qi, :] = 1.0
    c["c_oneq"] = oq.reshape(4, 512)
    return c


CONST_SHAPES = {
    "c_id": [128, 128], "c_mincl": [128, 128], "c_mstrict": [128, 128], "c_msuf": [128, 128],
    "c_ones": [128, 128], "c_cind": [128, 2], "c_iota": [128, 1], "c_caus": [128, 2048],
    "c_EE": [18, 2048], "c_qal": [2, 4096], "c_alb": [128, 256], "c_gtab": [128, 128],
    "c_own": [128, 128], "c_bias_s": [128, 1024], "c_caus_s": [4, 32], "c_oneq": [4, 512],
}


def build(stage=99):
    nc = bass.Bass("TRN2", target_bir_lowering=False)

    def din(name, shape, dt=F32):
        return nc.dram_tensor(name, list(shape), dt, kind="ExternalInput").ap()

    def dout(name, shape, dt=F32):
        return nc.dram_tensor(name, list(shape), dt, kind="ExternalOutput").ap()

    IN_SPECS = {
        "xT_pre": ([D, NPRE], F32), "xT_own": ([D, NTOK], F32), "x_own": ([NTOK, D], F32),
        "w_in": ([D, INW], F32), "w_pa": ([1024, D], F32), "w_pb": ([1024, D], F32), "w_out": ([D, D], F32),
        "lbl": ([2, 1024], F32), "rng": ([1, 1024], F32), "ln1": ([2, D], F32), "ln2": ([2, D], F32),
        "w_rt": ([D, 36], F32), "b_rt": ([1, 36], F32), "w1": ([32, D, 512], F32), "w3": ([32, D, 512], F32),
        "w2": ([32, 512, D], F32), "ck": ([163840, 1024], F32), "cv": ([163840, 1024], F32),
        "pt": ([1, 128], I32), "st": ([8, 128, 128], F32),
    }
    for n_, sh_ in CONST_SHAPES.items():
        IN_SPECS[n_] = (sh_, F32)
    _decl = {}

    class _Lazy:
        def __init__(self, name):
            self.name = name

        def ap(self):
            if self.name not in _decl:
                sh_, dt_ = IN_SPECS[self.name]
                _decl[self.name] = din(self.name, sh_, dt_)
            return _decl[self.name]

        def __getitem__(self, key):
            return self.ap()[key]

        def rearrange(self, *a, **kw):
            return self.ap().rearrange(*a, **kw)

    xT_pre, xT_own, x_own, w_in, w_pa, w_pb, w_out = [_Lazy(n_) for n_ in (
        "xT_pre", "xT_own", "x_own", "w_in", "w_pa", "w_pb", "w_out")]
    lbl, rng, ln1, ln2, w_rt, b_rt, w1, w3, w2, ck, cv, pt, st = [_Lazy(n_) for n_ in (
        "lbl", "rng", "ln1", "ln2", "w_rt", "b_rt", "w1", "w3", "w2", "ck", "cv", "pt", "st")]
    CD = {n_: _Lazy(n_) for n_ in CONST_SHAPES}
    nc._used_inputs = _decl

    o_y = dout("o_y", [NTOK, D])
    o_kv = dout("o_kv", [NTOK, 2048])
    o_recp = dout("o_recp", [8, 128, 128])
    o_recs = dout("o_recs", [8, 128, 128])

    tiles = [(i * 128, 128) for i in range(NT_OWN)] + [(NOWN, NS)]

    with ExitStack() as es:
        k = KB(nc, es)

        def SB(stack, shape, dt, name=None):
            k.nm += 1
            t = stack.enter_context(nc.sbuf_tensor(name or ("t%d" % k.nm), list(shape), dt))
            return Tile(t)

        PS = [k.psum([128, 512], F32, "ps%d" % i) for i in range(6)]
        PB = [k.psum([128, 512], BF16, "pb%d" % i) for i in range(2)]
        rot = {"lst": list(range(6)), "i": 0}

        def set_rot(lst):
            rot["lst"] = list(lst)
            rot["i"] = 0

        def nps():
            p = PS[rot["lst"][rot["i"] % len(rot["lst"])]]
            rot["i"] += 1
            return p

        pbi = [0]

        def npb():
            p = PB[pbi[0] % 2]
            pbi[0] += 1
            return p

        def cload(stack, name, dt=F32, q="sp"):
            sh = CONST_SHAPES[name]
            t = SB(stack, sh, dt, "s_" + name)
            k.dma(q if dt == F32 else "pool", t[:, :], CD[name].ap(), w=[t])
            return t

        idf = cload(es, "c_id")
        idb = SB(es, [128, 128], BF16, "idb")
        k.dma("pool", idb[:, :], CD["c_id"].ap(), w=[idb])
        onesf = cload(es, "c_ones")
        onesb = SB(es, [128, 128], BF16, "onesb")
        k.dma("pool", onesb[:, :], CD["c_ones"].ap(), w=[onesb])
        mincl = cload(es, "c_mincl")
        mstrict = cload(es, "c_mstrict")
        cind = cload(es, "c_cind")

        o_recT = SB(es, [128, 8, NTOK], BF16, "o_recT")
        o_attnT = SB(es, [128, 8, NTOK], BF16, "o_attnT")

        class Rot:
            def __init__(self, stack, n, shape, dt, name):
                self.ts = [SB(stack, shape, dt, "%s%d" % (name, i)) for i in range(n)]
                self.i = 0

            def get(self):
                t = self.ts[self.i % len(self.ts)]
                self.i += 1
                return t

        def load_w16(t, src_ap, ncols):
            v = src_ap.rearrange("(dc p) n -> p dc n", p=128)
            for h in range(2):
                k.dma("pool", t[:, 8 * h:8 * h + 8, 0:ncols], v[:, 8 * h:8 * h + 8, :], w=[t])

        def proj(xt, t0, P, wt, ncols, ps=None):
            ps = ps or nps()
            for dc in range(16):
                k.op("pe", lambda dc=dc: nc.tensor.matmul(ps[0:P, 0:ncols], xt[:, dc, t0:t0 + P], wt[:, dc, 0:ncols],
                                                       start=(dc == 0), stop=(dc == 15)),
                     r=[xt, wt], w=[ps])
            return ps

        def act(out, in_, func, r, w, **kw):
            return k.op("act", lambda: nc.scalar.activation(out=out, in_=in_, func=func, **kw), r=r, w=w)

        def vcopy(e, out, in_, r, w):
            if e == "act":
                return k.op("act", lambda: nc.scalar.copy(out=out, in_=in_), r=r, w=w)
            eng = nc.vector if e == "dve" else nc.gpsimd
            return k.op(e, lambda: eng.tensor_copy(out=out, in_=in_), r=r, w=w)

        def tt(out, in0, in1, op, r, w, e="dve"):
            eng = nc.vector if e == "dve" else nc.gpsimd
            return k.op(e, lambda: eng.tensor_tensor(out=out, in0=in0, in1=in1, op=op), r=r, w=w)

        def mm(ps_ap, lhsT, rhs, start, stop, r, w):
            return k.op("pe", lambda: nc.tensor.matmul(ps_ap, lhsT, rhs, start=start, stop=stop), r=r, w=w)

        def tr(ps_ap, in_, ident, r, w):
            return k.op("pe", lambda: nc.tensor.transpose(ps_ap, in_, ident), r=r, w=w)

        with ExitStack() as ph:
            xTo = SB(ph, [128, 16, NTOK], BF16, "xTo")
            xsrc = xT_own.rearrange("(dc p) t -> p dc t", p=128)
            for h in range(4):
                k.dma("pool", xTo[:, 4 * h:4 * h + 4, :], xsrc[:, 4 * h:4 * h + 4, :], w=[xTo])
            msuf = cload(ph, "c_msuf")
            LB = SB(ph, [128, 1024], F32, "LB")
            OML = SB(ph, [128, 1024], F32, "OML")
            RG = SB(ph, [128, 1024], F32, "RG")
            k.dma("sp", LB[:, :], lbl[0:1, :].partition_broadcast(128), w=[LB])
            k.dma("sp", OML[:, :], lbl[1:2, :].partition_broadcast(128), w=[OML])
            k.dma("sp", RG[:, :], rng[0:1, :].partition_broadcast(128), w=[RG])
            tt(LB[:, :], LB[:, :], OML[:, :], ALU.subtract, r=[LB, OML], w=[LB])
            act(LB[:, :], LB[:, :], AF.Sigmoid, r=[LB], w=[LB])
            k.op("dve", lambda: nc.vector.tensor_scalar(out=OML[:, :], in0=LB[:, :], scalar1=-1.0, scalar2=1.0,
                                                        op0=ALU.mult, op1=ALU.add), r=[LB], w=[OML])
            wts = [SB(ph, [128, 16, 512], BF16, "hw%d" % i) for i in range(4)]
            xtl = Rot(ph, 3, [128, 16, 128], BF16, "xtl")
            f32t = Rot(ph, 12, [128, 512], F32, "hf")
            bft = Rot(ph, 8, [128, 512], BF16, "hb")
            CB = SB(ph, [128, 512], F32, "CB")
            S = [SB(ph, [128, 128], F32, "S%d" % i) for i in range(4)]
            Sb = Rot(ph, 6, [128, 128], BF16, "Sb")
            smallb = Rot(ph, 8, [128, 128], BF16, "smb")
            QTa = [SB(ph, [128, 128], BF16, "QTa%d" % i) for i in range(2)]
            QTb = [SB(ph, [128, 128], BF16, "QTb%d" % i) for i in range(2)]
            for tq in QTa + QTb:
                k.op("dve", lambda tq=tq: nc.vector.memset(tq[:, :], 0.0), w=[tq])
            small = Rot(ph, 8, [128, 8], F32, "hsm")

            import os as _os
            _NG = int(_os.environ.get('HG_G', '2'))
            _NP = int(_os.environ.get('HG_NPRE', str(NT_PRE)))
            _NO = int(_os.environ.get('HG_NOWN', '9'))
            _CUT = float(_os.environ.get('HG_CUT', '99'))
            for g in range(_NG):
                set_rot([0, 1, 2, 3, 4])
                accS = PS[5]
                for i, off in enumerate((OFF_QR, OFF_FR, OFF_IR, OFF_OG)):
                    load_w16(wts[i], w_in[:, off + g * 512: off + (g + 1) * 512], 512)
                wq, wf, wi, wo = wts
                LBg = LB[:, g * 512:(g + 1) * 512]
                OMLg = OML[:, g * 512:(g + 1) * 512]
                RGg = RG[:, g * 512:(g + 1) * 512]
                k.op("dve", lambda: nc.vector.memset(CB[:, :], 0.0), w=[CB])

                def gates(psf, P):
                    sg = f32t.get()
                    act(sg[0:P, :], psf[0:P, :], AF.Sigmoid, r=[psf], w=[sg])
                    u = f32t.get()
                    tt(u[0:P, :], sg[0:P, :], OMLg[0:P, :], ALU.mult, r=[sg, OML], w=[u])
                    f = f32t.get()
                    tt(f[0:P, :], u[0:P, :], LBg[0:P, :], ALU.add, r=[u, LB], w=[f])
                    kk = f32t.get()
                    tt(kk[0:P, :], OMLg[0:P, :], u[0:P, :], ALU.subtract, r=[u, OML], w=[kk])
                    gl = f32t.get()
                    act(gl[0:P, :], f[0:P, :], AF.Ln, r=[f], w=[gl])
                    return kk, gl

                for j in list(reversed(range(NT_PRE)))[:_NP]:
                    xt = xtl.get()
                    xs = xT_pre[:, j * 128:(j + 1) * 128].rearrange("(dc p) t -> p dc t", p=128)
                    k.dma("pool", xt[:, :, :], xs, w=[xt])
                    psf = proj(xt, 0, 128, wf, 512)
                    psi = proj(xt, 0, 128, wi, 512)
                    kk, gl = gates(psf, 128)
                    Vi = bft.get()
                    vcopy("act", Vi[:, :], psi[:, :], r=[psi], w=[Vi])
                    psR = nps()
                    mm(psR[:, :], msuf[:, :], gl[:, :], True, False, r=[msuf, gl], w=[psR])
                    mm(psR[:, :], idf[:, :], CB[:, :], False, True, r=[idf, CB], w=[psR])
                    eR = f32t.get()
                    act(eR[:, :], psR[:, :], AF.Exp, r=[psR], w=[eR])
                    Kd = bft.get()
                    tt(Kd[:, :], kk[:, :], eR[:, :], ALU.mult, r=[kk, eR], w=[Kd])
                    for hh in range(4):
                        cs = slice(hh * 128, (hh + 1) * 128)
                        mm(accS[:, cs], Kd[:, cs], Vi[:, cs], (j == NT_PRE - 1 and hh == 0), (j == NT_PRE - _NP and hh == 3), r=[Kd, Vi], w=[accS])
                    if j > 0:
                        psC = nps()
                        mm(psC[:, :], onesf[:, :], gl[:, :], True, True, r=[onesf, gl], w=[psC])
                        tt(CB[:, :], CB[:, :], psC[:, :], ALU.add, r=[CB, psC], w=[CB])
                for hh in range(4):
                    vcopy("dve", S[hh][:, :], accS[:, hh * 128:(hh + 1) * 128], r=[accS], w=[S[hh]])

                for ti, (t0, P) in enumerate(tiles):
                    if ti >= _NO and P == 128:
                        continue
                    if P != 128 and _NO < 9 and _os.environ.get('HG_NOS'):
                        continue
                    two = (P == 128)
                    ca = 64 if two else P
                    if not two:
                        for hh in range(4):
                            k.dma("sp", o_recp[4 * g + hh, :, :], S[hh][:, :], r=[S[hh]], is_out=True)
                            k.dma("sp", S[hh][:, :], st[4 * g + hh, :, :], w=[S[hh]])
                    psq = proj(xTo, t0, P, wq, 512)
                    psf = proj(xTo, t0, P, wf, 512)
                    psi = proj(xTo, t0, P, wi, 512)
                    pso = proj(xTo, t0, P, wo, 512)
                    qr = f32t.get()
                    act(qr[0:P, :], psq[0:P, :], AF.Silu, r=[psq], w=[qr])
                    kk, gl = gates(psf, P)
                    Vi = bft.get()
                    vcopy("act", Vi[0:P, :], psi[0:P, :], r=[psi], w=[Vi])
                    so = f32t.get()
                    act(so[0:P, :], pso[0:P, :], AF.Sigmoid, r=[pso], w=[so])
                    tt(so[0:P, :], so[0:P, :], RGg[0:P, :], ALU.mult, r=[so, RG], w=[so])
                    if _CUT < 2:
                        continue
                    psG = nps()
                    mm(psG[0:P, :], mincl[0:P, 0:P], gl[0:P, :], True, True, r=[mincl, gl], w=[psG])
                    psR = nps()
                    mm(psR[0:P, :], mstrict[0:P, 0:P], gl[0:P, :], True, True, r=[mstrict, gl], w=[psR])
                    if _CUT < 1.3:
                        continue
                    eG = f32t.get()
                    act(eG[0:P, :], psG[0:P, :], AF.Exp, r=[psG], w=[eG])
                    enG = f32t.get()
                    act(enG[0:P, :], psG[0:P, :], AF.Exp, r=[psG], w=[enG], scale=-1.0)
                    eR = f32t.get()
                    act(eR[0:P, :], psR[0:P, :], AF.Exp, r=[psR], w=[eR])
                    if _CUT < 1.5:
                        continue
                    Qg = bft.get()
                    tt(Qg[0:P, :], qr[0:P, :], eG[0:P, :], ALU.mult, r=[qr, eG], w=[Qg])
                    Kg = bft.get()
                    tt(Kg[0:P, :], kk[0:P, :], enG[0:P, :], ALU.mult, r=[kk, enG], w=[Kg])
                    Kd = bft.get()
                    tt(Kd[0:P, :], kk[0:P, :], eR[0:P, :], ALU.mult, r=[kk, eR], w=[Kd])
                    if _CUT < 1.7:
                        continue
                    psL = nps()
                    ncol = 2 if two else 1
                    for hh in range(4):
                        for c_ in range(ncol):
                            mm(psL[:, c_ * 4 + hh:c_ * 4 + hh + 1], gl[0:P, hh * 128:(hh + 1) * 128],
                               cind[0:P, c_:c_ + 1], True, True, r=[gl, cind], w=[psL])
                    if _CUT < 1.9:
                        continue
                    dec = small.get()
                    act(dec[:, 0:4 * ncol], psL[:, 0:4 * ncol], AF.Exp, r=[psL], w=[dec])
                    for hh in range(4):
                        if _CUT < 3:
                            continue
                        h = 4 * g + hh
                        cs = slice(hh * 128, (hh + 1) * 128)
                        pT = npb()
                        tr(pT[:, 0:P], Qg[0:P, cs], idb[0:P, 0:P], r=[Qg, idb], w=[pT])
                        tr(pT[:, 128:128 + P], Kg[0:P, cs], idb[0:P, 0:P], r=[Kg, idb], w=[pT])
                        ce = "act" if hh % 2 == 0 else "dve"
                        QgT = smallb.get()
                        vcopy(ce, QgT[:, 0:P], pT[:, 0:P], r=[pT], w=[QgT])
                        KgT = smallb.get()
                        vcopy(ce, KgT[:, 0:P], pT[:, 128:128 + P], r=[pT], w=[KgT])
                        qa = QTa[hh % 2]
                        vcopy(ce, qa[:, 0:ca], pT[:, 0:ca], r=[pT], w=[qa])
                        if two:
                            qb = QTb[hh % 2]
                            vcopy(ce, qb[:, 64:128], pT[:, 64:128], r=[pT], w=[qb])
                        if _CUT < 4:
                            continue
                        psA = nps()
                        mm(psA[0:P, 0:P], KgT[:, 0:P], QgT[:, 0:P], True, True, r=[KgT, QgT], w=[psA])
                        Am = smallb.get()
                        tt(Am[0:P, 0:P], psA[0:P, 0:P], mincl[0:P, 0:P], ALU.mult, r=[psA, mincl], w=[Am])
                        Sa = Sb.get()
                        vcopy("act", Sa[:, :], S[hh][:, :], r=[S[hh]], w=[Sa])
                        psKa = nps()
                        mm(psKa[:, 0:128], Kd[0:ca, cs], Vi[0:ca, cs], True, True, r=[Kd, Vi], w=[psKa])
                        k.op("dve", lambda hh=hh, psKa=psKa, dec=dec: nc.vector.scalar_tensor_tensor(
                            out=S[hh][:, :], in0=S[hh][:, :], scalar=dec[:, hh:hh + 1], in1=psKa[:, 0:128],
                            op0=ALU.mult, op1=ALU.add), r=[S[hh], dec, psKa], w=[S[hh]])
                        if two:
                            Sbb = Sb.get()
                            vcopy("act", Sbb[:, :], S[hh][:, :], r=[S[hh]], w=[Sbb])
                            psKb = nps()
                            mm(psKb[:, 0:128], Kd[64:128, cs], Vi[64:128, cs], True, True, r=[Kd, Vi], w=[psKb])
                            k.op("dve", lambda hh=hh, psKb=psKb, dec=dec: nc.vector.scalar_tensor_tensor(
                                out=S[hh][:, :], in0=S[hh][:, :], scalar=dec[:, 4 + hh:5 + hh],
                                in1=psKb[:, 0:128], op0=ALU.mult, op1=ALU.add), r=[S[hh], dec, psKb], w=[S[hh]])
                        if _CUT < 5:
                            continue
                        psO = nps()
                        mm(psO[0:P, 0:128], Am[0:P, 0:P], Vi[0:P, cs], True, False, r=[Am, Vi], w=[psO])
                        mm(psO[0:P, 0:128], qa[:, 0:P], Sa[:, :], False, not two, r=[qa, Sa], w=[psO])
                        if two:
                            mm(psO[0:P, 0:128], qb[:, 0:P], Sbb[:, :], False, True, r=[qb, Sbb], w=[psO])
                        if _CUT < 6:
                            continue
                        junk = f32t.get()
                        ss = small.get()
                        osb = f32t.get()
                        vcopy("act", osb[0:P, 0:128], psO[0:P, 0:128], r=[psO], w=[osb])
                        act(junk[0:P, 0:128], osb[0:P, 0:128], AF.Square, r=[osb], w=[junk, ss], accum_out=ss[0:P, 0:1])
                        k.op("dve", lambda ss=ss, P=P: nc.vector.tensor_scalar(
                            out=ss[0:P, 1:2], in0=ss[0:P, 0:1], scalar1=1.0 / 128, scalar2=RMS_EPS,
                            op0=ALU.mult, op1=ALU.add), r=[ss], w=[ss])
                        act(ss[0:P, 3:4], ss[0:P, 1:2], AF.Sqrt, r=[ss], w=[ss])
                        k.op("dve", lambda ss=ss, P=P: nc.vector.reciprocal(out=ss[0:P, 2:3], in_=ss[0:P, 3:4]),
                             r=[ss], w=[ss])
                        orec = smallb.get()
                        k.op("dve", lambda ss=ss, P=P, orec=orec, osb=osb, so=so, cs=cs: nc.vector.scalar_tensor_tensor(
                            out=orec[0:P, :], in0=osb[0:P, 0:128], scalar=ss[0:P, 2:3], in1=so[0:P, cs],
                            op0=ALU.mult, op1=ALU.mult), r=[ss, osb, so], w=[orec])
                        pT2 = npb()
                        tr(pT2[:, 0:P], orec[0:P, :], idb[0:P, 0:P], r=[orec, idb], w=[pT2])
                        vcopy("act", o_recT[:, h, t0:t0 + P], pT2[:, 0:P], r=[pT2], w=[o_recT.sub((h, ti))])
                for hh in range(4):
                    k.dma("sp", o_recs[4 * g + hh, :, :], S[hh][:, :], r=[S[hh]], is_out=True)

        k.barrier()
        if stage <= 1:
            k.finish()
            return nc
        SCALE = 128.0 ** -0.5
        with ExitStack() as pa:
            caus = SB(pa, [128, 2048], BF16, "caus")
            k.dma("pool", caus[:, :], CD["c_caus"].ap(), w=[caus])
            EE = SB(pa, [18, 2048], BF16, "EE")
            k.dma("pool", EE[:, :], CD["c_EE"].ap(), w=[EE])
            alb = cload(pa, "c_alb")
            gtab = cload(pa, "c_gtab")
            owt = cload(pa, "c_own")
            QsT = SB(pa, [128, 8, 4], F32, "QsT")
            KnT = SB(pa, [128, 8, 4], F32, "KnT")
            Vn = SB(pa, [4, 1024], F32, "Vn")
            with ExitStack() as pg_:
                xTo = SB(pg_, [128, 16, NTOK], BF16, "xTo2")
                xsrc = xT_own.rearrange("(dc p) t -> p dc t", p=128)
                for h in range(4):
                    k.dma("pool", xTo[:, 4 * h:4 * h + 4, :], xsrc[:, 4 * h:4 * h + 4, :], w=[xTo])
                wq2 = SB(pg_, [128, 16, 256], BF16, "wq2")
                wk2 = SB(pg_, [128, 16, 256], BF16, "wk2")
                wv2 = SB(pg_, [128, 16, 256], BF16, "wv2")
                KT = SB(pg_, [128, 2, 4096 + NS], BF16, "KT")
                V = SB(pg_, [128, 33, 256], BF16, "Vt")
                QT = SB(pg_, [128, 2, NTOK], BF16, "QT")
                RH = [[SB(pg_, [18, 512], BF16, "RH%d%d" % (a_, b_)) for b_ in range(2)] for a_ in range(2)]
                xtl = Rot(pg_, 3, [128, 16, 128], BF16, "axt")
                f32a = Rot(pg_, 4, [128, 256], F32, "af")
                bfa = Rot(pg_, 4, [128, 256], BF16, "ab")
                PTs = Rot(pg_, 3, [128, 512], BF16, "PT")
                gsm = Rot(pg_, 12, [128, 16], F32, "gsm")
                gmb = Rot(pg_, 4, [128, 16], BF16, "gmb")
                ksb = SB(pg_, [128, 2, 16], BF16, "ksb")
                rDt = SB(pg_, [128, 512], F32, "rD")
                for g2 in range(4):
                    set_rot([0, 1, 2, 3])
                    psO, psD = PS[4], PS[5]
                    load_w16(wq2, w_in[:, OFF_QA + g2 * 256: OFF_QA + (g2 + 1) * 256], 256)
                    load_w16(wk2, w_in[:, OFF_KA + g2 * 256: OFF_KA + (g2 + 1) * 256], 256)
                    load_w16(wv2, w_in[:, OFF_VA + g2 * 256: OFF_VA + (g2 + 1) * 256], 256)
                    for hh in range(2):
                        for qg in range(2):
                            h = 2 * g2 + hh
                            k.dma("pool", RH[hh][qg][16:18, :], CD["c_qal"].ap()[:, h * 512:(h + 1) * 512], w=[RH[hh][qg]])
                    for ti, (t0, P) in enumerate(tiles):
                        psq = proj(xTo, t0, P, wq2, 256)
                        psk = proj(xTo, t0, P, wk2, 256)
                        psv = proj(xTo, t0, P, wv2, 256)
                        qb = bfa.get()
                        act(qb[0:P, :], psq[0:P, 0:256], AF.Copy, r=[psq], w=[qb], scale=SCALE)
                        kf = f32a.get()
                        vcopy("act", kf[0:P, :], psk[0:P, 0:256], r=[psk], w=[kf])
                        vf = f32a.get()
                        vcopy("dve", vf[0:P, :], psv[0:P, 0:256], r=[psv], w=[vf])
                        k.dma("sp", o_kv[t0:t0 + P, g2 * 256:(g2 + 1) * 256], kf[0:P, :], r=[kf], is_out=True)
                        k.dma("sp", o_kv[t0:t0 + P, 1024 + g2 * 256:1024 + (g2 + 1) * 256], vf[0:P, :], r=[vf], is_out=True)
                        kb = bfa.get()
                        vcopy("dve", kb[0:P, :], kf[0:P, :], r=[kf], w=[kb])
                        vcopy("dve", V[0:P, 24 + ti, :], vf[0:P, :], r=[vf], w=[V.sub(24 + ti)])
                        if P != 128:
                            vcopy("act", Vn[0:P, g2 * 256:(g2 + 1) * 256], vf[0:P, :], r=[vf], w=[Vn])
                        for hh in range(2):
                            h = 2 * g2 + hh
                            cs = slice(hh * 128, (hh + 1) * 128)
                            pT = npb()
                            tr(pT[:, 0:P], qb[0:P, cs], idb[0:P, 0:P], r=[qb, idb], w=[pT])
                            tr(pT[:, 128:128 + P], kb[0:P, cs], idb[0:P, 0:P], r=[kb, idb], w=[pT])
                            ce = "act" if hh == 0 else "dve"
                            vcopy(ce, QT[:, hh, t0:t0 + P], pT[:, 0:P], r=[pT], w=[QT.sub((hh, ti))])
                            vcopy(ce, KT[:, hh, NPRE + t0:NPRE + t0 + P], pT[:, 128:128 + P], r=[pT], w=[KT.sub((hh, 24 + ti))])
                            if P != 128:
                                vcopy(ce, QsT[:, h, :], pT[:, 0:P], r=[pT], w=[QsT])
                                vcopy(ce, KnT[:, h, :], pT[:, 128:128 + P], r=[pT], w=[KnT])
                    for j in range(NT_PRE):
                        xt = xtl.get()
                        xs = xT_pre[:, j * 128:(j + 1) * 128].rearrange("(dc p) t -> p dc t", p=128)
                        k.dma("pool", xt[:, :, :], xs, w=[xt])
                        psk = proj(xt, 0, 128, wk2, 256)
                        psv = proj(xt, 0, 128, wv2, 256)
                        kb = bfa.get()
                        vcopy("act", kb[:, :], psk[:, 0:256], r=[psk], w=[kb])
                        vcopy("dve", V[:, j, :], psv[:, 0:256], r=[psv], w=[V.sub(j)])
                        pT = npb()
                        tr(pT[:, 0:128], kb[:, 0:128], idb[:, :], r=[kb, idb], w=[pT])
                        tr(pT[:, 128:256], kb[:, 128:256], idb[:, :], r=[kb, idb], w=[pT])
                        ce = "act" if j % 2 == 0 else "dve"
                        vcopy(ce, KT[:, 0, j * 128:(j + 1) * 128], pT[:, 0:128], r=[pT], w=[KT.sub((0, j))])
                        vcopy(ce, KT[:, 1, j * 128:(j + 1) * 128], pT[:, 128:256], r=[pT], w=[KT.sub((1, j))])
                    KTall = [[KT.sub((hh, j)) for j in range(33)] for hh in range(2)]
                    Vall = [V.sub(j) for j in range(33)]
                    QTall = [[QT.sub((hh, i)) for i in range(9)] for hh in range(2)]
                    for hh in range(2):
                        ks = gsm.get()
                        k.op("dve", lambda hh=hh, ks=ks: nc.vector.tensor_reduce(
                            out=ks[:, 0:16], in_=KT[:, hh, 0:4096].rearrange("p (n s) -> p n s", s=256),
                            axis=AX.X, op=ALU.add), r=KTall[hh], w=[ks])
                        vcopy("dve", ksb[:, hh, :], ks[:, 0:16], r=[ks], w=[ksb])
                    for hh in range(2):
                        for i in range(NT_OWN):
                            psg = nps()
                            mm(psg[:, 0:16], QT[:, hh, i * 128:(i + 1) * 128], ksb[:, hh, :], True, True,
                               r=[QTall[hh][i], ksb], w=[psg])
                            g1 = gsm.get()
                            tt(g1[:, :], psg[:, 0:16], gtab[:, i * 16:(i + 1) * 16], ALU.add, r=[psg, gtab], w=[g1])
                            mx = gsm.get()
                            k.op("dve", lambda mx=mx, g1=g1: nc.vector.max(out=mx[:, 0:8], in_=g1[:, :]), r=[g1], w=[mx])
                            sel = gsm.get()
                            k.op("dve", lambda sel=sel, g1=g1, mx=mx: nc.vector.tensor_scalar(
                                out=sel[:, :], in0=g1[:, :], scalar1=mx[:, 2:3], scalar2=None, op0=ALU.is_ge),
                                r=[g1, mx], w=[sel])
                            val = gsm.get()
                            k.op("dve", lambda val=val, g1=g1: nc.vector.tensor_scalar(
                                out=val[:, :], in0=g1[:, :], scalar1=-1.0e29, scalar2=None, op0=ALU.is_gt),
                                r=[g1], w=[val])
                            tt(sel[:, :], sel[:, :], val[:, :], ALU.mult, r=[sel, val], w=[sel])
                            tt(sel[:, :], sel[:, :], owt[:, i * 16:(i + 1) * 16], ALU.max, r=[sel, owt], w=[sel])
                            mb = gmb.get()
                            k.op("dve", lambda mb=mb, sel=sel: nc.vector.tensor_scalar(
                                out=mb[:, :], in0=sel[:, :], scalar1=-1.0, scalar2=1.0e30, op0=ALU.add, op1=ALU.mult),
                                r=[sel], w=[mb])
                            pT = npb()
                            tr(pT[0:16, 0:128], mb[:, 0:16], idb[:, :], r=[mb, idb], w=[pT])
                            rh = RH[hh][i // 4]
                            vcopy("act", rh[0:16, (i % 4) * 128:(i % 4 + 1) * 128], pT[0:16, 0:128], r=[pT], w=[rh])
                    for hh in range(2):
                        h = 2 * g2 + hh
                        for qg in range(2):
                            nkt = 24 + 4 * qg + 4
                            qsl = slice(qg * 512, (qg + 1) * 512)
                            qdeps = QTall[hh][4 * qg:4 * qg + 4]
                            rh = RH[hh][qg]
                            for kt in range(nkt):
                                m = kt - (24 + 4 * qg)
                                psS = nps()
                                mm(psS[:, :], KT[:, hh, kt * 128:(kt + 1) * 128], QT[:, hh, qsl], True, False,
                                   r=[KTall[hh][kt]] + qdeps, w=[psS])
                                mm(psS[:, :], EE[0:18, (kt // 2) * 128:(kt // 2 + 1) * 128], rh[0:18, :], False, m < 0,
                                   r=[EE, rh], w=[psS])
                                if m >= 0:
                                    mm(psS[:, :], idb[:, :], caus[:, m * 512:(m + 1) * 512], False, True,
                                       r=[idb, caus], w=[psS])
                                PT = PTs.get()
                                col = h * 32 + (m + 28)
                                act(PT[:, :], psS[:, :], AF.Exp, r=[psS, alb], w=[PT], bias=alb[:, col:col + 1])
                                mm(psO[:, :], V[:, kt, hh * 128:(hh + 1) * 128], PT[:, :], kt == 0, kt == nkt - 1,
                                   r=[Vall[kt], PT], w=[psO])
                                mm(psD[:, :], onesb[:, :], PT[:, :], kt == 0, kt == nkt - 1, r=[onesb, PT], w=[psD])
                            k.op("dve", lambda: nc.vector.reciprocal(out=rDt[:, :], in_=psD[:, :]), r=[psD], w=[rDt])
                            tt(o_attnT[:, h, qsl], psO[:, :], rDt[:, :], ALU.mult, r=[psO, rDt], w=[o_attnT.sub((h, qg))])

            k.barrier()
            if stage <= 2:
                k.finish()
                return nc

            with ExitStack() as psm:
                set_rot([0, 1, 2, 3])
                psKS, psOs = PS[4], PS[5]
                iota = cload(psm, "c_iota")
                bias_s = cload(psm, "c_bias_s")
                caus_s = cload(psm, "c_caus_s")
                oneq = cload(psm, "c_oneq")
                ptb = SB(psm, [128, 128], I32, "ptb")
                k.dma("sp", ptb[:, :], pt.ap().partition_broadcast(128), w=[ptb])
                ptf = SB(psm, [128, 128], F32, "ptf")
                vcopy("dve", ptf[:, :], ptb[:, :], r=[ptb], w=[ptf])
                k.op("dve", lambda: nc.vector.tensor_scalar(out=ptf[:, :], in0=ptf[:, :], scalar1=128.0,
                                                            scalar2=iota[:, 0:1], op0=ALU.mult, op1=ALU.add),
                     r=[ptf, iota], w=[ptf])
                idx = SB(psm, [128, 128], I32, "idx")
                vcopy("dve", idx[:, :], ptf[:, :], r=[ptf], w=[idx])
                Sall = SB(psm, [128, 8, 4, 2, 64], F32, "Sall")
                pages = Rot(psm, 3, [128, 1024], F32, "pg")
                kTs = Rot(psm, 2, [128, 1024], F32, "kTs")
                for pg in range(128):
                    n, a = divmod(pg, 2)
                    kp = pages.get()
                    k.gather(kp[:, :], ck.ap(), idx[:, pg:pg + 1], r=[idx], w=[kp])
                    for h in range(8):
                        mm(psKS[:, h * 64 + n:h * 64 + n + 1], kp[:, h * 128:(h + 1) * 128], onesf[:, 0:1],
                           pg == 0 and h == 0, pg == 127 and h == 7, r=[kp, onesf], w=[psKS])
                    kT = kTs.get()
                    for half in range(2):
                        pTf = nps()
                        for hq in range(4):
                            h = half * 4 + hq
                            mm(pTf[:, hq * 128:(hq + 1) * 128], kp[:, h * 128:(h + 1) * 128], idf[:, :], True, True,
                               r=[kp, idf], w=[pTf])
                        vcopy("act" if half == 0 else "dve", kT[:, half * 512:(half + 1) * 512], pTf[:, :],
                              r=[pTf], w=[kT.sub(half)])
                    psSc = nps()
                    for h in range(8):
                        mm(psSc[:, h * 4:(h + 1) * 4], kT[:, h * 128:(h + 1) * 128], QsT[:, h, :], True, True,
                           r=[kT.sub(h // 4), QsT], w=[psSc])
                    vcopy("act", Sall[:, :, :, a, n], psSc[:, 0:32].rearrange("p (h q) -> p h q", q=4),
                          r=[psSc], w=[Sall])
                ksT = SB(psm, [128, 512], F32, "ksT")
                vcopy("dve", ksT[:, :], psKS[:, :], r=[psKS], w=[ksT])
                psGt = nps()
                for h in range(8):
                    mm(psGt[0:4, h * 64:(h + 1) * 64], QsT[:, h, :], ksT[:, h * 64:(h + 1) * 64], True, True,
                       r=[QsT, ksT], w=[psGt])
                gsb = SB(psm, [4, 512], F32, "gsb")
                vcopy("dve", gsb[:, :], psGt[0:4, :], r=[psGt], w=[gsb])
                mx8 = SB(psm, [4, 64], F32, "mx8")
                mbs = SB(psm, [4, 512], F32, "mbs")
                for h in range(8):
                    hs = slice(h * 64, (h + 1) * 64)
                    k.op("dve", lambda h=h, hs=hs: nc.vector.max(out=mx8[:, h * 8:(h + 1) * 8], in_=gsb[:, hs]),
                         r=[gsb], w=[mx8])
                    k.op("dve", lambda h=h, hs=hs: nc.vector.tensor_scalar(
                        out=mbs[:, hs], in0=gsb[:, hs], scalar1=mx8[:, h * 8 + 2:h * 8 + 3], scalar2=None,
                        op0=ALU.is_ge), r=[gsb, mx8], w=[mbs])
                k.op("dve", lambda: nc.vector.tensor_scalar(out=mbs[:, :], in0=mbs[:, :], scalar1=-1.0, scalar2=1.0e30,
                                                            op0=ALU.add, op1=ALU.mult), r=[mbs], w=[mbs])
                for q in range(4):
                    psB = nps()
                    mm(psB[:, :], oneq[0:4, q * 128:(q + 1) * 128], mbs[0:4, :], True, True, r=[oneq, mbs], w=[psB])
                    for a in range(2):
                        tt(Sall[:, :, q, a, :], Sall[:, :, q, a, :], psB[:, :].rearrange("p (h n) -> p h n", n=64),
                           ALU.add, r=[Sall, psB], w=[Sall])
                    tt(Sall[:, :, q, :, :], Sall[:, :, q, :, :],
                       bias_s[:, :].rearrange("p (h a n) -> p h a n", a=2, n=64), ALU.add, r=[Sall, bias_s], w=[Sall])
                for h in range(8):
                    act(Sall[:, h, :, :, :], Sall[:, h, :, :, :], AF.Exp, r=[Sall], w=[Sall])
                psOw = nps()
                for h in range(8):
                    mm(psOw[0:4, h * 4:(h + 1) * 4], KnT[:, h, :], QsT[:, h, :], True, True, r=[KnT, QsT], w=[psOw])
                pown = SB(psm, [4, 32], F32, "pown")
                tt(pown[:, :], psOw[0:4, 0:32], caus_s[:, :], ALU.add, r=[psOw, caus_s], w=[pown])
                act(pown[:, :], pown[:, :], AF.Exp, r=[pown], w=[pown])
                psDs = nps()
                for pg in range(128):
                    n, a = divmod(pg, 2)
                    vp = pages.get()
                    k.gather(vp[:, :], cv.ap(), idx[:, pg:pg + 1], r=[idx], w=[vp])
                    for h in range(8):
                        mm(psOs[:, h * 4:(h + 1) * 4], vp[:, h * 128:(h + 1) * 128], Sall[:, h, :, a, n],
                           pg == 0 and h == 0, False, r=[vp, Sall], w=[psOs])
                    mm(psDs[:, 0:32], onesf[:, :], Sall[:, :, :, a, n], pg == 0, False, r=[onesf, Sall], w=[psDs])
                for h in range(8):
                    mm(psOs[:, h * 4:(h + 1) * 4], Vn[0:4, h * 128:(h + 1) * 128], pown[0:4, h * 4:(h + 1) * 4],
                       False, h == 7, r=[Vn, pown], w=[psOs])
                mm(psDs[:, 0:32], onesf[0:4, :], pown[0:4, :], False, True, r=[onesf, pown], w=[psDs])
                rDs = SB(psm, [128, 32], F32, "rDs")
                k.op("dve", lambda: nc.vector.reciprocal(out=rDs[:, :], in_=psDs[:, 0:32]), r=[psDs], w=[rDs])
                tt(o_attnT[:, :, NOWN:NOWN + NS], psOs[:, 0:32].rearrange("p (h q) -> p h q", q=4),
                   rDs[:, :].rearrange("p (h q) -> p h q", q=4), ALU.mult, r=[psOs, rDs], w=[o_attnT.sub("s")])

        k.barrier()
        if stage <= 3:
            k.finish()
            return nc
        with ExitStack() as pm:
            set_rot([0, 1, 2, 3, 4, 5])
            mergedT = SB(pm, [128, 16, NTOK], BF16, "mergedT")
            with ExitStack() as pm1:
                xTo = SB(pm1, [128, 16, NTOK], BF16, "xTo3")
                xsrc = xT_own.rearrange("(dc p) t -> p dc t", p=128)
                for h in range(4):
                    k.dma("pool", xTo[:, 4 * h:4 * h + 4, :], xsrc[:, 4 * h:4 * h + 4, :], w=[xTo])
                wpa = SB(pm1, [128, 8, 512], BF16, "wpa")
                wpb = SB(pm1, [128, 8, 512], BF16, "wpb")
                wga = SB(pm1, [128, 16, 512], BF16, "wga")
                wgb = SB(pm1, [128, 16, 512], BF16, "wgb")
                mf = Rot(pm1, 6, [128, 512], F32, "mf")
                mgb = Rot(pm1, 2, [128, 512], BF16, "mgb")
                for dmb in range(4):
                    dsl = slice(dmb * 512, (dmb + 1) * 512)
                    k.dma("pool", wpa[:, :, :], w_pa[:, dsl].rearrange("(h p) n -> p h n", p=128), w=[wpa])
                    k.dma("pool", wpb[:, :, :], w_pb[:, dsl].rearrange("(h p) n -> p h n", p=128), w=[wpb])
                    load_w16(wga, w_in[:, OFF_GA + dmb * 512:OFF_GA + (dmb + 1) * 512], 512)
                    load_w16(wgb, w_in[:, OFF_GB + dmb * 512:OFF_GB + (dmb + 1) * 512], 512)
                    for ti, (t0, P) in enumerate(tiles):
                        psA = nps()
                        for h in range(8):
                            mm(psA[0:P, :], o_attnT[:, h, t0:t0 + P], wpa[:, h, :], h == 0, h == 7,
                               r=[o_attnT.sub((h, 0)), o_attnT.sub((h, 1)), o_attnT.sub("s"), wpa], w=[psA])
                        psB = nps()
                        for h in range(8):
                            mm(psB[0:P, :], o_recT[:, h, t0:t0 + P], wpb[:, h, :], h == 0, h == 7,
                               r=[o_recT.sub((h, ti)), wpb], w=[psB])
                        psga = proj(xTo, t0, P, wga, 512)
                        psgb = proj(xTo, t0, P, wgb, 512)
                        sga = mf.get()
                        act(sga[0:P, :], psga[0:P, :], AF.Sigmoid, r=[psga], w=[sga])
                        sgb = mf.get()
                        act(sgb[0:P, :], psgb[0:P, :], AF.Sigmoid, r=[psgb], w=[sgb])
                        m1 = mf.get()
                        tt(m1[0:P, :], sga[0:P, :], psA[0:P, :], ALU.mult, r=[sga, psA], w=[m1])
                        tt(sgb[0:P, :], sgb[0:P, :], psB[0:P, :], ALU.mult, r=[sgb, psB], w=[sgb])
                        mg = mgb.get()
                        tt(mg[0:P, :], m1[0:P, :], sgb[0:P, :], ALU.add, r=[m1, sgb], w=[mg])
                        pT = npb()
                        for j in range(4):
                            tr(pT[:, j * 128:j * 128 + P], mg[0:P, j * 128:(j + 1) * 128], idb[0:P, 0:P], r=[mg, idb], w=[pT])
                        vcopy("act", mergedT[:, dmb * 4:(dmb + 1) * 4, t0:t0 + P],
                              pT[:, :].rearrange("p (j t) -> p j t", t=128)[:, :, 0:P], r=[pT], w=[mergedT.sub((dmb, ti))])
            k.barrier()
            MT = [[mergedT.sub((d_, t_)) for d_ in range(4)] for t_ in range(9)]
            if stage <= 4:
                k.finish()
                return nc
            buf = SB(pm, [128, 9, 2048], F32, "buf")
            for ti, (t0, P) in enumerate(tiles):
                k.dma("sp", buf[0:P, ti, :], x_own[t0:t0 + P, :], w=[buf.sub(ti)])
            lnsm = Rot(pm, 6, [128, 32], F32, "lnsm")

            def layer_norm_tiles(lnp, after):
                with ExitStack() as pl:
                    k.barrier()
                    Gt = SB(pl, [128, 2048], F32)
                    Bt = SB(pl, [128, 2048], F32)
                    k.dma("sp", Gt[:, :], lnp[0:1, :].partition_broadcast(128), w=[Gt])
                    k.dma("sp", Bt[:, :], lnp[1:2, :].partition_broadcast(128), w=[Bt])
                    for ti, (t0, P) in enumerate(tiles):
                        bt = buf.sub(ti)
                        st_ = lnsm.get()
                        for c_ in range(4):
                            k.op("dve", lambda c_=c_, st_=st_, ti=ti, P=P: nc.vector.bn_stats(
                                out=st_[0:P, c_ * 6:(c_ + 1) * 6], in_=buf[0:P, ti, c_ * 512:(c_ + 1) * 512]),
                                r=[bt], w=[st_])
                        mv = lnsm.get()
                        k.op("dve", lambda st_=st_, mv=mv, P=P: nc.vector.bn_aggr(
                            out=mv[0:P, 0:2], in_=st_[0:P, 0:24].rearrange("p (c s) -> p c s", s=6)), r=[st_], w=[mv])
                        k.op("dve", lambda mv=mv, P=P: nc.vector.tensor_scalar(
                            out=mv[0:P, 2:3], in0=mv[0:P, 1:2], scalar1=LN_EPS, scalar2=None, op0=ALU.add),
                            r=[mv], w=[mv])
                        act(mv[0:P, 3:4], mv[0:P, 2:3], AF.Sqrt, r=[mv], w=[mv])
                        k.op("dve", lambda mv=mv, P=P: nc.vector.reciprocal(out=mv[0:P, 4:5], in_=mv[0:P, 3:4]),
                             r=[mv], w=[mv])
                        k.op("dve", lambda mv=mv, P=P, ti=ti: nc.vector.tensor_scalar(
                            out=buf[0:P, ti, :], in0=buf[0:P, ti, :], scalar1=mv[0:P, 0:1], scalar2=mv[0:P, 4:5],
                            op0=ALU.subtract, op1=ALU.mult), r=[mv, bt], w=[bt])
                        tt(buf[0:P, ti, :], buf[0:P, ti, :], Gt[0:P, :], ALU.mult, r=[bt, Gt], w=[bt])
                        tt(buf[0:P, ti, :], buf[0:P, ti, :], Bt[0:P, :], ALU.add, r=[bt, Bt], w=[bt])
                        after(ti, t0, P)

            with ExitStack() as pm2:
                wos = Rot(pm2, 2, [128, 16, 512], BF16, "wo")
                for dmb in range(4):
                    wo_ = wos.get()
                    load_w16(wo_, w_out[:, dmb * 512:(dmb + 1) * 512], 512)
                    for ti, (t0, P) in enumerate(tiles):
                        ps = nps()
                        for dc in range(16):
                            mm(ps[0:P, :], mergedT[:, dc, t0:t0 + P], wo_[:, dc, :], dc == 0, dc == 15,
                               r=[MT[ti][dc // 4], wo_], w=[ps])
                        k.op("dve", lambda ps=ps, ti=ti, P=P, dmb=dmb: nc.vector.scalar_tensor_tensor(
                            out=buf[0:P, ti, dmb * 512:(dmb + 1) * 512], in0=buf[0:P, ti, dmb * 512:(dmb + 1) * 512],
                            scalar=ALPHA, in1=ps[0:P, :], op0=ALU.mult, op1=ALU.add), r=[ps, buf.sub(ti)], w=[buf.sub(ti)])
            k.barrier()
            x1T = mergedT
            cgate = SB(pm, [128, 9, 32], F32, "cgate")
            wrt = SB(pm, [128, 16, 36], BF16, "wrt")
            k.dma("pool", wrt[:, :, :], w_rt.rearrange("(dc p) n -> p dc n", p=128), w=[wrt])
            brt = SB(pm, [128, 36], F32, "brt")
            k.dma("sp", brt[:, :], b_rt[0:1, :].partition_broadcast(128), w=[brt])
            x1b = Rot(pm, 2, [128, 2048], BF16, "x1b")
            rsm = Rot(pm, 16, [128, 36], F32, "rsm")

            def after_ln1(ti, t0, P):
                bt = buf.sub(ti)
                xb = x1b.get()
                vcopy("act", xb[0:P, :], buf[0:P, ti, :], r=[bt], w=[xb])
                for q4 in range(4):
                    pT = npb()
                    for j in range(4):
                        dc = q4 * 4 + j
                        tr(pT[:, j * 128:j * 128 + P], xb[0:P, dc * 128:(dc + 1) * 128], idb[0:P, 0:P], r=[xb, idb], w=[pT])
                    vcopy("act" if q4 % 2 == 0 else "dve", x1T[:, q4 * 4:(q4 + 1) * 4, t0:t0 + P],
                          pT[:, :].rearrange("p (j t) -> p j t", t=128)[:, :, 0:P], r=[pT], w=[x1T.sub((q4, ti))])
                k.op("dve", lambda: nc.vector.tensor_scalar(out=buf[0:P, ti, :], in0=buf[0:P, ti, :], scalar1=ALPHA,
                                                            scalar2=None, op0=ALU.mult), r=[bt], w=[bt])
                psr = nps()
                for dc in range(16):
                    mm(psr[0:P, 0:36], x1T[:, dc, t0:t0 + P], wrt[:, dc, :], dc == 0, dc == 15,
                       r=[x1T.sub((dc // 4, ti)), wrt], w=[psr])
                lg = rsm.get()
                tt(lg[0:P, :], psr[0:P, 0:36], brt[0:P, :], ALU.add, r=[psr, brt], w=[lg])
                g8 = rsm.get()
                k.op("dve", lambda: nc.vector.memset(g8[0:P, 0:8], NEG), w=[g8])
                vcopy("dve", g8[0:P, 0:4], lg[0:P, 0:4], r=[lg], w=[g8])
                mx = rsm.get()
                k.op("dve", lambda: nc.vector.max(out=mx[0:P, 0:8], in_=g8[0:P, 0:8]), r=[g8], w=[mx])
                oh = rsm.get()
                k.op("dve", lambda: nc.vector.tensor_scalar(out=oh[0:P, 0:4], in0=lg[0:P, 0:4], scalar1=mx[0:P, 0:1],
                                                            scalar2=None, op0=ALU.is_ge), r=[lg, mx], w=[oh])
                k.op("dve", lambda: nc.vector.tensor_scalar(out=mx[0:P, 8:9], in0=mx[0:P, 0:1], scalar1=-1.0,
                                                            scalar2=None, op0=ALU.mult), r=[mx], w=[mx])
                eg = rsm.get()
                act(eg[0:P, 0:4], lg[0:P, 0:4], AF.Exp, r=[lg, mx], w=[eg], bias=mx[0:P, 8:9], accum_out=eg[0:P, 8:9])
                k.op("dve", lambda: nc.vector.reciprocal(out=eg[0:P, 9:10], in_=eg[0:P, 8:9]), r=[eg], w=[eg])
                el = rsm.get()
                k.op("dve", lambda: nc.vector.tensor_scalar(out=el[0:P, 0:8], in0=lg[0:P, 4:12], scalar1=oh[0:P, 0:1],
                                                            scalar2=None, op0=ALU.mult), r=[lg, oh], w=[el])
                for g_ in range(1, 4):
                    k.op("dve", lambda g_=g_: nc.vector.scalar_tensor_tensor(
                        out=el[0:P, 0:8], in0=lg[0:P, 4 + 8 * g_:12 + 8 * g_], scalar=oh[0:P, g_:g_ + 1],
                        in1=el[0:P, 0:8], op0=ALU.mult, op1=ALU.add), r=[lg, oh, el], w=[el])
                m2 = rsm.get()
                k.op("dve", lambda: nc.vector.max(out=m2[0:P, 0:8], in_=el[0:P, 0:8]), r=[el], w=[m2])
                se = rsm.get()
                k.op("dve", lambda: nc.vector.tensor_scalar(out=se[0:P, 0:8], in0=el[0:P, 0:8], scalar1=m2[0:P, 1:2],
                                                            scalar2=None, op0=ALU.is_ge), r=[el, m2], w=[se])
                k.op("dve", lambda: nc.vector.tensor_scalar(out=m2[0:P, 8:9], in0=m2[0:P, 0:1], scalar1=-1.0,
                                                            scalar2=None, op0=ALU.mult), r=[m2], w=[m2])
                ex = rsm.get()
                act(ex[0:P, 0:8], el[0:P, 0:8], AF.Exp, r=[el, m2], w=[ex], bias=m2[0:P, 8:9])
                tt(ex[0:P, 0:8], ex[0:P, 0:8], se[0:P, 0:8], ALU.mult, r=[ex, se], w=[ex])
                k.op("dve", lambda: nc.vector.tensor_reduce(out=ex[0:P, 8:9], in_=ex[0:P, 0:8], axis=AX.X, op=ALU.add),
                     r=[ex], w=[ex])
                k.op("dve", lambda: nc.vector.reciprocal(out=ex[0:P, 9:10], in_=ex[0:P, 8:9]), r=[ex], w=[ex])
                tt(ex[0:P, 9:10], ex[0:P, 9:10], eg[0:P, 9:10], ALU.mult, r=[ex, eg], w=[ex])
                k.op("dve", lambda: nc.vector.tensor_scalar(out=ex[0:P, 0:8], in0=ex[0:P, 0:8], scalar1=ex[0:P, 9:10],
                                                            scalar2=None, op0=ALU.mult), r=[ex], w=[ex])
                for g_ in range(4):
                    k.op("dve", lambda g_=g_: nc.vector.tensor_scalar(
                        out=cgate[0:P, ti, 8 * g_:8 * g_ + 8], in0=ex[0:P, 0:8], scalar1=oh[0:P, g_:g_ + 1],
                        scalar2=None, op0=ALU.mult), r=[ex, oh], w=[cgate])

            layer_norm_tiles(ln1, after_ln1)
            k.barrier()
            if stage <= 5:
                k.finish()
                return nc
            X1 = [[x1T.sub((q4, ti)) for q4 in range(4)] for ti in range(9)]
            with ExitStack() as pm3:
                class _Alias:
                    def __init__(self, src):
                        self.src = src
                        self.b = Buf()
                        for sb_ in src.subs.values():
                            self.b.r.extend(sb_.r)
                            if sb_.w is not None:
                                self.b.r.append(sb_.w)
                        self.v = src[:, :, :].rearrange("p h t -> p (h t)")

                    def __getitem__(self, key):
                        return self.v[key]

                class _WE:
                    def __init__(self):
                        self.ts = [_Alias(o_recT), _Alias(o_attnT), SB(pm3, [128, 8192], BF16, "wE2"), SB(pm3, [128, 8192], BF16, "wE3")]
                        self.i = 0

                    def get(self):
                        t = self.ts[self.i % 4]
                        self.i += 1
                        return t
                wE = _WE()
                hid = SB(pm3, [128, 4, NTOK], BF16, "hid")
                s1t = Rot(pm3, 3, [128, 512], F32, "s1t")
                groups = [(0, 512), (512, 512), (1024, NS)]
                import os as _os2
                NE = int(_os2.environ.get("MOE_NE", "32"))
                for e in range(NE):
                    w1t = wE.get()
                    w3t = wE.get()
                    w2t = wE.get()
                    v1 = w1[e].rearrange("(dc p) f -> p dc f", p=128)
                    v3 = w3[e].rearrange("(dc p) f -> p dc f", p=128)
                    v2 = w2[e].rearrange("(fc p) d -> p fc d", p=128)
                    w1v = w1t[:, 0:8192].rearrange("p (dc f) -> p dc f", f=512)
                    w3v = w3t[:, 0:8192].rearrange("p (dc f) -> p dc f", f=512)
                    w2v = w2t[:, 0:8192].rearrange("p (fc d) -> p fc d", d=2048)
                    for hf in range(2):
                        k.dma("pool", w1v[:, 8 * hf:8 * hf + 8, :], v1[:, 8 * hf:8 * hf + 8, :], w=[w1t])
                    for hf in range(2):
                        k.dma("pool", w3v[:, 8 * hf:8 * hf + 8, :], v3[:, 8 * hf:8 * hf + 8, :], w=[w3t])
                    for hf in range(2):
                        k.dma("pool", w2v[:, 2 * hf:2 * hf + 2, :], v2[:, 2 * hf:2 * hf + 2, :], w=[w2t])
                    for gi, (c0, n) in enumerate(groups):
                        xdeps = [X1[ti][q4] for ti in range(9) for q4 in range(4) if (tiles[ti][0] >= c0 and tiles[ti][0] < c0 + n)]
                        for fc in range(4):
                            ps1 = nps()
                            for dc in range(16):
                                mm(ps1[:, 0:n], w1v[:, dc, fc * 128:(fc + 1) * 128], x1T[:, dc, c0:c0 + n], dc == 0, dc == 15,
                                   r=[w1t] + xdeps, w=[ps1])
                            ps3 = nps()
                            for dc in range(16):
                                mm(ps3[:, 0:n], w3v[:, dc, fc * 128:(fc + 1) * 128], x1T[:, dc, c0:c0 + n], dc == 0, dc == 15,
                                   r=[w3t] + xdeps, w=[ps3])
                            s1 = s1t.get()
                            act(s1[:, 0:n], ps1[:, 0:n], AF.Silu, r=[ps1], w=[s1])
                            tt(hid[:, fc, c0:c0 + n], s1[:, 0:n], ps3[:, 0:n], ALU.mult, r=[s1, ps3], w=[hid.sub((fc, gi))])
                    for ti, (t0, P) in enumerate(tiles):
                        gi = 0 if t0 < 512 else (1 if t0 < 1024 else 2)
                        for dmb in range(4):
                            psy = nps()
                            for fc in range(4):
                                mm(psy[0:P, :], hid[:, fc, t0:t0 + P], w2v[:, fc, dmb * 512:(dmb + 1) * 512], fc == 0, fc == 3,
                                   r=[hid.sub((fc, gi)), w2t], w=[psy])
                            k.op("dve", lambda psy=psy, ti=ti, P=P, dmb=dmb, e=e: nc.vector.scalar_tensor_tensor(
                                out=buf[0:P, ti, dmb * 512:(dmb + 1) * 512], in0=psy[0:P, :], scalar=cgate[0:P, ti, e:e + 1],
                                in1=buf[0:P, ti, dmb * 512:(dmb + 1) * 512], op0=ALU.mult, op1=ALU.add),
                                r=[psy, cgate, buf.sub(ti)], w=[buf.sub(ti)])

            k.barrier()
            def after_ln2(ti, t0, P):
                k.dma("sp", o_y[t0:t0 + P, :], buf[0:P, ti, :], r=[buf.sub(ti)], is_out=True)

            layer_norm_tiles(ln2, after_ln2)
        k.finish()
    return nc


def _prep_inputs(inputs):
    f = lambda n: np.asarray(inputs[n], dtype=np.float32)
    x_prompt = f("x_prompt")
    x_sample = f("x_sample")
    shared = {
        "w_in": np.ascontiguousarray(f("w_in")[0]),
        "w_pa": np.ascontiguousarray(f("w_pa")[0]),
        "w_pb": np.ascontiguousarray(f("w_pb")[0]),
        "w_out": np.ascontiguousarray(f("w_out")[0]),
        "lbl": np.ascontiguousarray(f("lb_logits")),
        "rng": np.ascontiguousarray(f("rec_norm_g")),
        "ln1": np.ascontiguousarray(np.concatenate([f("ln1_g"), f("ln1_b")], 0)),
        "ln2": np.ascontiguousarray(np.concatenate([f("ln2_g"), f("ln2_b")], 0)),
        "w_rt": np.ascontiguousarray(np.concatenate(
            [f("w_group")[0], f("w_router")[0].transpose(1, 0, 2).reshape(D, 32)], 1)),
        "b_rt": np.ascontiguousarray(np.concatenate(
            [f("b_group")[0].reshape(1, 4), f("b_router")[0].reshape(1, 32)], 1)),
        "w1": np.ascontiguousarray(f("w1")[0]),
        "w3": np.ascontiguousarray(f("w3")[0]),
        "w2": np.ascontiguousarray(f("w2")[0]),
        "ck": np.ascontiguousarray(f("cache_k")[0]).reshape(163840, 1024),
        "cv": np.ascontiguousarray(f("cache_v")[0]).reshape(163840, 1024),
    }
    page_table = np.asarray(inputs["page_table"]).astype(np.int32)
    state_rec = f("state_rec")[0]
    maps = []
    for c in range(8):
        b, qt = divmod(c, 4)
        xp = x_prompt[b]
        pre = np.zeros((NPRE, D), np.float32)
        n_pre = qt * 1024
        if n_pre:
            pre[NPRE - n_pre:] = xp[:n_pre]
        own = np.concatenate([xp[qt * 1024:(qt + 1) * 1024], x_sample[c]], 0)
        m = dict(shared)
        m.update({
            "xT_pre": np.ascontiguousarray(pre.T),
            "xT_own": np.ascontiguousarray(own.T),
            "x_own": np.ascontiguousarray(own),
            "pt": np.ascontiguousarray(page_table[c].reshape(1, 128)),
            "st": np.ascontiguousarray(state_rec[c]),
        })
        m.update(_host_consts(qt))
        maps.append(m)
    return maps


def _run(inputs, stage=99):
    nc = build(stage)
    maps = _prep_inputs(inputs)
    used = set(nc._used_inputs.keys())
    maps = [{n: v for n, v in m.items() if n in used} for m in maps]
    res = run_bass_kernel_spmd(nc, maps, core_ids=list(range(8)))
    return res


def kernel(**inputs):
    res = _run(inputs)
    R = res.results
    y_prompt = np.zeros((2, 4096, D), np.float32)
    y_sample = np.zeros((8, 4, D), np.float32)
    k_prompt = np.zeros((1, 2, 4096, 8, 128), np.float32)
    v_prompt = np.zeros((1, 2, 4096, 8, 128), np.float32)
    rec_prompt = np.zeros((1, 2, 8, 128, 128), np.float32)
    k_sample = np.zeros((1, 8, 4, 8, 128), np.float32)
    v_sample = np.zeros((1, 8, 4, 8, 128), np.float32)
    rec_sample = np.zeros((1, 8, 8, 128, 128), np.float32)
    for c in range(8):
        b, qt = divmod(c, 4)
        sl = slice(qt * 1024, (qt + 1) * 1024)
        oy = np.asarray(R[c]["o_y"])
        okv = np.asarray(R[c]["o_kv"])
        y_prompt[b, sl] = oy[:1024]
        y_sample[c] = oy[1024:]
        k_prompt[0, b, sl] = okv[:1024, :1024].reshape(1024, 8, 128)
        v_prompt[0, b, sl] = okv[:1024, 1024:].reshape(1024, 8, 128)
        k_sample[0, c] = okv[1024:, :1024].reshape(4, 8, 128)
        v_sample[0, c] = okv[1024:, 1024:].reshape(4, 8, 128)
        rec_sample[0, c] = np.asarray(R[c]["o_recs"])
        if qt == 3:
            rec_prompt[0, b] = np.asarray(R[c]["o_recp"])
    return (y_prompt, y_sample, k_prompt, v_prompt, rec_prompt, k_sample, v_sample, rec_sample)
```

```python
import numpy as np
from contextlib import ExitStack
import concourse.bass as bass
import concourse.mybir as mybir
from concourse.bass_utils import run_bass_kernel_spmd

F32 = mybir.dt.float32
BF16 = mybir.dt.bfloat16
I32 = mybir.dt.int32
U32 = mybir.dt.uint32
AF = mybir.ActivationFunctionType
ALU = mybir.AluOpType
AX = mybir.AxisListType

D = 2048
NPRE = 3072
NOWN = 1024
NS = 4
NTOK = NOWN + NS
NT_PRE = NPRE // 128
NT_OWN = NOWN // 128
INW = 11264
OFF_QA, OFF_KA, OFF_VA, OFF_QR, OFF_FR, OFF_IR, OFF_OG, OFF_GA, OFF_GB = (
    0, 1024, 2048, 3072, 4096, 5120, 6144, 7168, 9216)
NEG = -1.0e30
ALPHA = 2.0 ** 0.25
LN_EPS = 1e-5
RMS_EPS = 1e-6
NDS = 24


class Buf:
    __slots__ = ("w", "r")

    def __init__(self):
        self.w = None
        self.r = []


class Tile:
    def __init__(self, t):
        self.t = t
        self.b = Buf()
        self.subs = {}

    def __getitem__(self, k):
        return self.t[k]

    def sub(self, key):
        if key not in self.subs:
            self.subs[key] = Buf()
        return self.subs[key]


def _bufs(xs):
    out = []
    for x in xs:
        if isinstance(x, Buf):
            out.append(x)
        else:
            out.append(x.b)
    return out


class KB:
    def __init__(self, nc, es):
        self.nc = nc
        self.es = es
        self.E = {"pe": nc.tensor, "act": nc.scalar, "dve": nc.vector, "pool": nc.gpsimd, "sp": nc.sync}
        self.psem = {e: es.enter_context(nc.semaphore("p_" + e)) for e in self.E}
        self.pcnt = {e: 0 for e in self.E}
        self.seen = {e: {} for e in self.E}
        self.dsem = [es.enter_context(nc.semaphore("d%d" % i)) for i in range(NDS)]
        self.dcnt = [0] * NDS
        self.dnext = {"sw": 0, "hw": NDS // 2}
        self.nm = 0
        self.out_toks = []

    def sb(self, shape, dt, name=None):
        self.nm += 1
        t = self.es.enter_context(self.nc.sbuf_tensor(name or ("t%d" % self.nm), list(shape), dt))
        return Tile(t)

    def psum(self, shape, dt, name=None):
        self.nm += 1
        t = self.es.enter_context(self.nc.psum_tensor(name or ("p%d" % self.nm), list(shape), dt))
        return Tile(t)

    def _slot(self, kind):
        h = NDS // 2
        s = self.dnext[kind]
        base = 0 if kind == "sw" else h
        self.dnext[kind] = base + (s - base + 1) % h
        return s

    def _sem(self, key):
        return self.psem[key[1]] if key[0] == "p" else self.dsem[key[1]]

    def _wait(self, e, deps):
        best = {}
        for key, val in deps:
            if key == ("p", "pe") and e == "pe":
                continue
            if val > best.get(key, 0):
                best[key] = val
        for key, val in best.items():
            if self.seen[e].get(key, 0) >= val:
                continue
            self.E[e].wait_ge(self._sem(key), val)
            self.seen[e][key] = val

    def _deps(self, r, w):
        deps = []
        for b in r:
            if b.w is not None:
                deps.append(b.w)
        for b in w:
            if b.w is not None:
                deps.append(b.w)
            deps.extend(b.r)
        return deps

    def _commit(self, tok, r, w):
        for b in r:
            b.r.append(tok)
        for b in w:
            b.w = tok
            b.r = []

    def op(self, e, fn, r=(), w=()):
        r = _bufs(r)
        w = _bufs(w)
        self._wait(e, self._deps(r, w))
        ins = fn()
        self.pcnt[e] += 1
        ins.then_inc(self.psem[e], 1)
        tok = (("p", e), self.pcnt[e])
        self._commit(tok, r, w)
        return tok

    def dma(self, q, out, in_, r=(), w=(), is_out=False):
        r = _bufs(r)
        w = _bufs(w)
        self._wait(q, self._deps(r, w))
        s = self._slot("sw" if q == "pool" else "hw")
        if self.dcnt[s] > 0:
            self._wait(q, [(("d", s), self.dcnt[s])])
        ins = self.E[q].dma_start(out=out, in_=in_)
        self.dcnt[s] += 16
        ins.then_inc(self.dsem[s], 16)
        tok = (("d", s), self.dcnt[s])
        self._commit(tok, r, w)
        if is_out:
            self.out_toks.append(tok)
        return tok

    def gather(self, out, in_, idx_ap, r=(), w=()):
        q = "pool"
        r = _bufs(r)
        w = _bufs(w)
        self._wait(q, self._deps(r, w))
        s = self._slot("sw")
        if self.dcnt[s] > 0:
            self._wait(q, [(("d", s), self.dcnt[s])])
        ins = self.nc.gpsimd.indirect_dma_start(
            out=out, out_offset=None, in_=in_,
            in_offset=bass.IndirectOffsetOnAxis(ap=idx_ap, axis=0))
        self.dcnt[s] += 16
        ins.then_inc(self.dsem[s], 16)
        tok = (("d", s), self.dcnt[s])
        self._commit(tok, r, w)
        return tok

    def barrier(self):
        toks = [(("p", e), self.pcnt[e]) for e in self.E if self.pcnt[e] > 0]
        toks += [(("d", s), self.dcnt[s]) for s in range(NDS) if self.dcnt[s] > 0]
        for e in self.E:
            self._wait(e, [t for t in toks if t[0] != ("p", e)])

    def finish(self):
        self._wait("sp", self.out_toks)
        self._wait("sp", [(("p", e), self.pcnt[e]) for e in self.E if e != "sp" and self.pcnt[e] > 0])


def _host_consts(qt):
    c = {}
    idn = np.eye(128, dtype=np.float32)
    s = np.arange(128)[:, None]
    t = np.arange(128)[None, :]
    same = (s // 64) == (t // 64)
    c["c_id"] = idn
    c["c_mincl"] = (same & (s <= t)).astype(np.float32)
    c["c_mstrict"] = (same & (s > t)).astype(np.float32)
    c["c_msuf"] = (s > t).astype(np.float32)
    c["c_ones"] = np.ones((128, 128), np.float32)
    cind = np.zeros((128, 2), np.float32)
    cind[:64, 0] = 1
    cind[64:, 1] = 1
    c["c_cind"] = cind
    c["c_iota"] = np.arange(128, dtype=np.float32).reshape(128, 1)
    slopes = 2.0 ** (-np.arange(1, 9, dtype=np.float64))
    q = np.arange(512)[None, None, :]
    tt = np.arange(128)[:, None, None]
    jj = np.arange(4)[None, :, None]
    c["c_caus"] = np.where(q >= 128 * jj + tt, 0.0, NEG).astype(np.float32).reshape(128, 2048)
    ee = np.zeros((18, 16, 128), np.float32)
    for n in range(16):
        ee[n, n, :] = 1.0
    ee[16:18] = 1.0
    c["c_EE"] = ee.reshape(18, 2048)
    qq = np.arange(512)
    qal = np.zeros((2, 8, 512), np.float64)
    for h in range(8):
        qal[0, h] = -slopes[h] * 128 * (qq // 128)
        qal[1, h] = -slopes[h] * (qq % 128)
    c["c_qal"] = qal.reshape(2, 4096).astype(np.float32)
    alb = np.zeros((128, 8, 32), np.float64)
    for h in range(8):
        for mi in range(32):
            alb[:, h, mi] = slopes[h] * (128 * (mi - 28) + np.arange(128))
    c["c_alb"] = alb.reshape(128, 256).astype(np.float32)
    gt = np.zeros((8, 16), np.float32)
    ow = np.zeros((8, 16), np.float32)
    for i in range(8):
        n_own = 12 + i // 2
        for n in range(16):
            ok = (n < n_own) and (n >= 12 - 4 * qt)
            gt[i, n] = 0.0 if ok else NEG
        ow[i, n_own] = 1.0
    c["c_gtab"] = np.broadcast_to(gt.reshape(1, 128), (128, 128)).copy()
    c["c_own"] = np.broadcast_to(ow.reshape(1, 128), (128, 128)).copy()
    bs = np.zeros((128, 8, 2, 64), np.float64)
    for h in range(8):
        for a in range(2):
            for n in range(64):
                bs[:, h, a, n] = slopes[h] * (128 * (2 * n + a) + np.arange(128) - 16384)
    c["c_bias_s"] = bs.reshape(128, 1024).astype(np.float32)
    cs = np.zeros((4, 8, 4), np.float64)
    for tk in range(4):
        for h in range(8):
            for qi in range(4):
                cs[tk, h, qi] = slopes[h] * tk if tk <= qi else NEG
    c["c_caus_s"] = cs.reshape(4, 32).astype(np.float32)
    oq = np.zeros((4, 4, 128), np.float32)
    for qi in range(4):
        oq[qi, qi, :] = 1.0
    c["c_oneq"] = oq.reshape(4, 512)
    return c


CONST_SHAPES = {
    "c_id": [128, 128], "c_mincl": [128, 128], "c_mstrict": [128, 128], "c_msuf": [128, 128],
    "c_ones": [128, 128], "c_cind": [128, 2], "c_iota": [128, 1], "c_caus": [128, 2048],
    "c_EE": [18, 2048], "c_qal": [2, 4096], "c_alb": [128, 256], "c_gtab": [128, 128],
    "c_own": [128, 128], "c_bias_s": [128, 1024], "c_caus_s": [4, 32], "c_oneq": [4, 512],
}


def build(stage=99):
    nc = bass.Bass("TRN2", target_bir_lowering=False)

    def din(name, shape, dt=F32):
        return nc.dram_tensor(name, list(shape), dt, kind="ExternalInput").ap()

    def dout(name, shape, dt=F32):
        return nc.dram_tensor(name, list(shape), dt, kind="ExternalOutput").ap()

    IN_SPECS = {
        "xT_pre": ([D, NPRE], F32), "xT_own": ([D, NTOK], F32), "x_own": ([NTOK, D], F32),
        "w_in": ([D, INW], F32), "w_pa": ([1024, D], F32), "w_pb": ([1024, D], F32), "w_out": ([D, D], F32),
        "lbl": ([2, 1024], F32), "rng": ([1, 1024], F32), "ln1": ([2, D], F32), "ln2": ([2, D], F32),
        "w_rt": ([D, 36], F32), "b_rt": ([1, 36], F32), "w1": ([32, D, 512], F32), "w3": ([32, D, 512], F32),
        "w2": ([32, 512, D], F32), "ck": ([163840, 1024], F32), "cv": ([163840, 1024], F32),
        "pt": ([1, 128], I32), "st": ([8, 128, 128], F32),
    }
    for n_, sh_ in CONST_SHAPES.items():
        IN_SPECS[n_] = (sh_, F32)
    _decl = {}

    class _Lazy:
        def __init__(self, name):
            self.name = name

        def ap(self):
            if self.name not in _decl:
                sh_, dt_ = IN_SPECS[self.name]
                _decl[self.name] = din(self.name, sh_, dt_)
            return _decl[self.name]

        def __getitem__(self, key):
            return self.ap()[key]

        def rearrange(self, *a, **kw):
            return self.ap().rearrange(*a, **kw)

    xT_pre, xT_own, x_own, w_in, w_pa, w_pb, w_out = [_Lazy(n_) for n_ in (
        "xT_pre", "xT_own", "x_own", "w_in", "w_pa", "w_pb", "w_out")]
    lbl, rng, ln1, ln2, w_rt, b_rt, w1, w3, w2, ck, cv, pt, st = [_Lazy(n_) for n_ in (
        "lbl", "rng", "ln1", "ln2", "w_rt", "b_rt", "w1", "w3", "w2", "ck", "cv", "pt", "st")]
    CD = {n_: _Lazy(n_) for n_ in CONST_SHAPES}
    nc._used_inputs = _decl

    o_y = dout("o_y", [NTOK, D])
    o_kv = dout("o_kv", [NTOK, 2048])
    o_recp = dout("o_recp", [8, 128, 128])
    o_recs = dout("o_recs", [8, 128, 128])

    tiles = [(i * 128, 128) for i in range(NT_OWN)] + [(NOWN, NS)]

    with ExitStack() as es:
        k = KB(nc, es)

        def SB(stack, shape, dt, name=None):
            k.nm += 1
            t = stack.enter_context(nc.sbuf_tensor(name or ("t%d" % k.nm), list(shape), dt))
            return Tile(t)

        PS = [k.psum([128, 512], F32, "ps%d" % i) for i in range(6)]
        PB = [k.psum([128, 512], BF16, "pb%d" % i) for i in range(2)]
        rot = {"lst": list(range(6)), "i": 0}

        def set_rot(lst):
            rot["lst"] = list(lst)
            rot["i"] = 0

        def nps():
            p = PS[rot["lst"][rot["i"] % len(rot["lst"])]]
            rot["i"] += 1
            return p

        pbi = [0]

        def npb():
            p = PB[pbi[0] % 2]
            pbi[0] += 1
            return p

        def cload(stack, name, dt=F32, q="sp"):
            sh = CONST_SHAPES[name]
            t = SB(stack, sh, dt, "s_" + name)
            k.dma(q if dt == F32 else "pool", t[:, :], CD[name].ap(), w=[t])
            return t

        idf = cload(es, "c_id")
        idb = SB(es, [128, 128], BF16, "idb")
        k.dma("pool", idb[:, :], CD["c_id"].ap(), w=[idb])
        onesf = cload(es, "c_ones")
        onesb = SB(es, [128, 128], BF16, "onesb")
        k.dma("pool", onesb[:, :], CD["c_ones"].ap(), w=[onesb])
        mincl = cload(es, "c_mincl")
        mstrict = cload(es, "c_mstrict")
        cind = cload(es, "c_cind")

        o_recT = SB(es, [128, 8, NTOK], BF16, "o_recT")
        o_attnT = SB(es, [128, 8, NTOK], BF16, "o_attnT")

        class Rot:
            def __init__(self, stack, n, shape, dt, name):
                self.ts = [SB(stack, shape, dt, "%s%d" % (name, i)) for i in range(n)]
                self.i = 0

            def get(self):
                t = self.ts[self.i % len(self.ts)]
                self.i += 1
                return t

        def load_w16(t, src_ap, ncols):
            v = src_ap.rearrange("(dc p) n -> p dc n", p=128)
            for h in range(2):
                k.dma("pool", t[:, 8 * h:8 * h + 8, 0:ncols], v[:, 8 * h:8 * h + 8, :], w=[t])

        def proj(xt, t0, P, wt, ncols, ps=None):
            ps = ps or nps()
            for dc in range(16):
                k.op("pe", lambda dc=dc: nc.tensor.matmul(ps[0:P, 0:ncols], xt[:, dc, t0:t0 + P], wt[:, dc, 0:ncols],
                                                       start=(dc == 0), stop=(dc == 15)),
                     r=[xt, wt], w=[ps])
            return ps

        def act(out, in_, func, r, w, **kw):
            return k.op("act", lambda: nc.scalar.activation(out=out, in_=in_, func=func, **kw), r=r, w=w)

        def vcopy(e, out, in_, r, w):
            if e == "act":
                return k.op("act", lambda: nc.scalar.copy(out=out, in_=in_), r=r, w=w)
            eng = nc.vector if e == "dve" else nc.gpsimd
            return k.op(e, lambda: eng.tensor_copy(out=out, in_=in_), r=r, w=w)

        def tt(out, in0, in1, op, r, w, e="dve"):
            eng = nc.vector if e == "dve" else nc.gpsimd
            return k.op(e, lambda: eng.tensor_tensor(out=out, in0=in0, in1=in1, op=op), r=r, w=w)

        def mm(ps_ap, lhsT, rhs, start, stop, r, w):
            return k.op("pe", lambda: nc.tensor.matmul(ps_ap, lhsT, rhs, start=start, stop=stop), r=r, w=w)

        def tr(ps_ap, in_, ident, r, w):
            return k.op("pe", lambda: nc.tensor.transpose(ps_ap, in_, ident), r=r, w=w)

        with ExitStack() as ph:
            xTo = SB(ph, [128, 16, NTOK], BF16, "xTo")
            xsrc = xT_own.rearrange("(dc p) t -> p dc t", p=128)
            for h in range(4):
                k.dma("pool", xTo[:, 4 * h:4 * h + 4, :], xsrc[:, 4 * h:4 * h + 4, :], w=[xTo])
            msuf = cload(ph, "c_msuf")
            LB = SB(ph, [128, 1024], F32, "LB")
            OML = SB(ph, [128, 1024], F32, "OML")
            RG = SB(ph, [128, 1024], F32, "RG")
            k.dma("sp", LB[:, :], lbl[0:1, :].partition_broadcast(128), w=[LB])
            k.dma("sp", OML[:, :], lbl[1:2, :].partition_broadcast(128), w=[OML])
            k.dma("sp", RG[:, :], rng[0:1, :].partition_broadcast(128), w=[RG])
            tt(LB[:, :], LB[:, :], OML[:, :], ALU.subtract, r=[LB, OML], w=[LB])
            act(LB[:, :], LB[:, :], AF.Sigmoid, r=[LB], w=[LB])
            k.op("dve", lambda: nc.vector.tensor_scalar(out=OML[:, :], in0=LB[:, :], scalar1=-1.0, scalar2=1.0,
                                                        op0=ALU.mult, op1=ALU.add), r=[LB], w=[OML])
            wts = [SB(ph, [128, 16, 512], BF16, "hw%d" % i) for i in range(4)]
            xtl = Rot(ph, 3, [128, 16, 128], BF16, "xtl")
            f32t = Rot(ph, 12, [128, 512], F32, "hf")
            bft = Rot(ph, 8, [128, 512], BF16, "hb")
            CB = SB(ph, [128, 512], F32, "CB")
            S = [SB(ph, [128, 128], F32, "S%d" % i) for i in range(4)]
            Sb = Rot(ph, 6, [128, 128], BF16, "Sb")
            smallb = Rot(ph, 8, [128, 128], BF16, "smb")
            QTa = [SB(ph, [128, 128], BF16, "QTa%d" % i) for i in range(2)]
            QTb = [SB(ph, [128, 128], BF16, "QTb%d" % i) for i in range(2)]
            for tq in QTa + QTb:
                k.op("dve", lambda tq=tq: nc.vector.memset(tq[:, :], 0.0), w=[tq])
            small = Rot(ph, 8, [128, 8], F32, "hsm")

            import os as _os
            _NG = int(_os.environ.get('HG_G', '2'))
            _NP = int(_os.environ.get('HG_NPRE', str(NT_PRE)))
            _NO = int(_os.environ.get('HG_NOWN', '9'))
            _CUT = float(_os.environ.get('HG_CUT', '99'))
            for g in range(_NG):
                set_rot([0, 1, 2, 3, 4])
                accS = PS[5]
                for i, off in enumerate((OFF_QR, OFF_FR, OFF_IR, OFF_OG)):
                    load_w16(wts[i], w_in[:, off + g * 512: off + (g + 1) * 512], 512)
                wq, wf, wi, wo = wts
                LBg = LB[:, g * 512:(g + 1) * 512]
                OMLg = OML[:, g * 512:(g + 1) * 512]
                RGg = RG[:, g * 512:(g + 1) * 512]
                k.op("dve", lambda: nc.vector.memset(CB[:, :], 0.0), w=[CB])

                def gates(psf, P):
                    sg = f32t.get()
                    act(sg[0:P, :], psf[0:P, :], AF.Sigmoid, r=[psf], w=[sg])
                    u = f32t.get()
                    tt(u[0:P, :], sg[0:P, :], OMLg[0:P, :], ALU.mult, r=[sg, OML], w=[u])
                    f = f32t.get()
                    tt(f[0:P, :], u[0:P, :], LBg[0:P, :], ALU.add, r=[u, LB], w=[f])
                    kk = f32t.get()
                    tt(kk[0:P, :], OMLg[0:P, :], u[0:P, :], ALU.subtract, r=[u, OML], w=[kk])
                    gl = f32t.get()
                    act(gl[0:P, :], f[0:P, :], AF.Ln, r=[f], w=[gl])
                    return kk, gl

                for j in list(reversed(range(NT_PRE)))[:_NP]:
                    xt = xtl.get()
                    xs = xT_pre[:, j * 128:(j + 1) * 128].rearrange("(dc p) t -> p dc t", p=128)
                    k.dma("pool", xt[:, :, :], xs, w=[xt])
                    psf = proj(xt, 0, 128, wf, 512)
                    psi = proj(xt, 0, 128, wi, 512)
                    kk, gl = gates(psf, 128)
                    Vi = bft.get()
                    vcopy("act", Vi[:, :], psi[:, :], r=[psi], w=[Vi])
                    psR = nps()
                    mm(psR[:, :], msuf[:, :], gl[:, :], True, False, r=[msuf, gl], w=[psR])
                    mm(psR[:, :], idf[:, :], CB[:, :], False, True, r=[idf, CB], w=[psR])
                    eR = f32t.get()
                    act(eR[:, :], psR[:, :], AF.Exp, r=[psR], w=[eR])
                    Kd = bft.get()
                    tt(Kd[:, :], kk[:, :], eR[:, :], ALU.mult, r=[kk, eR], w=[Kd])
                    for hh in range(4):
                        cs = slice(hh * 128, (hh + 1) * 128)
                        mm(accS[:, cs], Kd[:, cs], Vi[:, cs], (j == NT_PRE - 1 and hh == 0), (j == NT_PRE - _NP and hh == 3), r=[Kd, Vi], w=[accS])
                    if j > 0:
                        psC = nps()
                        mm(psC[:, :], onesf[:, :], gl[:, :], True, True, r=[onesf, gl], w=[psC])
                        tt(CB[:, :], CB[:, :], psC[:, :], ALU.add, r=[CB, psC], w=[CB])
                for hh in range(4):
                    vcopy("dve", S[hh][:, :], accS[:, hh * 128:(hh + 1) * 128], r=[accS], w=[S[hh]])

                for ti, (t0, P) in enumerate(tiles):
                    if ti >= _NO and P == 128:
                        continue
                    if P != 128 and _NO < 9 and _os.environ.get('HG_NOS'):
                        continue
                    two = (P == 128)
                    ca = 64 if two else P
                    if not two:
                        for hh in range(4):
                            k.dma("sp", o_recp[4 * g + hh, :, :], S[hh][:, :], r=[S[hh]], is_out=True)
                            k.dma("sp", S[hh][:, :], st[4 * g + hh, :, :], w=[S[hh]])
                    psq = proj(xTo, t0, P, wq, 512)
                    psf = proj(xTo, t0, P, wf, 512)
                    psi = proj(xTo, t0, P, wi, 512)
                    pso = proj(xTo, t0, P, wo, 512)
                    qr = f32t.get()
                    act(qr[0:P, :], psq[0:P, :], AF.Silu, r=[psq], w=[qr])
                    kk, gl = gates(psf, P)
                    Vi = bft.get()
                    vcopy("act", Vi[0:P, :], psi[0:P, :], r=[psi], w=[Vi])
                    so = f32t.get()
                    act(so[0:P, :], pso[0:P, :], AF.Sigmoid, r=[pso], w=[so])
                    tt(so[0:P, :], so[0:P, :], RGg[0:P, :], ALU.mult, r=[so, RG], w=[so])
                    if _CUT < 2:
                        continue
                    psG = nps()
                    mm(psG[0:P, :], mincl[0:P, 0:P], gl[0:P, :], True, True, r=[mincl, gl], w=[psG])
                    psR = nps()
                    mm(psR[0:P, :], mstrict[0:P, 0:P], gl[0:P, :], True, True, r=[mstrict, gl], w=[psR])
                    if _CUT < 1.3:
                        continue
                    eG = f32t.get()
                    act(eG[0:P, :], psG[0:P, :], AF.Exp, r=[psG], w=[eG])
                    enG = f32t.get()
                    act(enG[0:P, :], psG[0:P, :], AF.Exp, r=[psG], w=[enG], scale=-1.0)
                    eR = f32t.get()
                    act(eR[0:P, :], psR[0:P, :], AF.Exp, r=[psR], w=[eR])
                    if _CUT < 1.5:
                        continue
                    Qg = bft.get()
                    tt(Qg[0:P, :], qr[0:P, :], eG[0:P, :], ALU.mult, r=[qr, eG], w=[Qg])
                    Kg = bft.get()
                    tt(Kg[0:P, :], kk[0:P, :], enG[0:P, :], ALU.mult, r=[kk, enG], w=[Kg])
                    Kd = bft.get()
                    tt(Kd[0:P, :], kk[0:P, :], eR[0:P, :], ALU.mult, r=[kk, eR], w=[Kd])
                    if _CUT < 1.7:
                        continue
                    psL = nps()
                    ncol = 2 if two else 1
                    for hh in range(4):
                        for c_ in range(ncol):
                            mm(psL[:, c_ * 4 + hh:c_ * 4 + hh + 1], gl[0:P, hh * 128:(hh + 1) * 128],
                               cind[0:P, c_:c_ + 1], True, True, r=[gl, cind], w=[psL])
                    if _CUT < 1.9:
                        continue
                    dec = small.get()
                    act(dec[:, 0:4 * ncol], psL[:, 0:4 * ncol], AF.Exp, r=[psL], w=[dec])
                    for hh in range(4):
                        if _CUT < 3:
                            continue
                        h = 4 * g + hh
                        cs = slice(hh * 128, (hh + 1) * 128)
                        pT = npb()
                        tr(pT[:, 0:P], Qg[0:P, cs], idb[0:P, 0:P], r=[Qg, idb], w=[pT])
                        tr(pT[:, 128:128 + P], Kg[0:P, cs], idb[0:P, 0:P], r=[Kg, idb], w=[pT])
                        ce = "act" if hh % 2 == 0 else "dve"
                        QgT = smallb.get()
                        vcopy(ce, QgT[:, 0:P], pT[:, 0:P], r=[pT], w=[QgT])
                        KgT = smallb.get()
                        vcopy(ce, KgT[:, 0:P], pT[:, 128:128 + P], r=[pT], w=[KgT])
                        qa = QTa[hh % 2]
                        vcopy(ce, qa[:, 0:ca], pT[:, 0:ca], r=[pT], w=[qa])
                        if two:
                            qb = QTb[hh % 2]
                            vcopy(ce, qb[:, 64:128], pT[:, 64:128], r=[pT], w=[qb])
                        if _CUT < 4:
                            continue
                        psA = nps()
                        mm(psA[0:P, 0:P], KgT[:, 0:P], QgT[:, 0:P], True, True, r=[KgT, QgT], w=[psA])
                        Am = smallb.get()
                        tt(Am[0:P, 0:P], psA[0:P, 0:P], mincl[0:P, 0:P], ALU.mult, r=[psA, mincl], w=[Am])
                        Sa = Sb.get()
                        vcopy("act", Sa[:, :], S[hh][:, :], r=[S[hh]], w=[Sa])
                        psKa = nps()
                        mm(psKa[:, 0:128], Kd[0:ca, cs], Vi[0:ca, cs], True, True, r=[Kd, Vi], w=[psKa])
                        k.op("dve", lambda hh=hh, psKa=psKa, dec=dec: nc.vector.scalar_tensor_tensor(
                            out=S[hh][:, :], in0=S[hh][:, :], scalar=dec[:, hh:hh + 1], in1=psKa[:, 0:128],
                            op0=ALU.mult, op1=ALU.add), r=[S[hh], dec, psKa], w=[S[hh]])
                        if two:
                            Sbb = Sb.get()
                            vcopy("act", Sbb[:, :], S[hh][:, :], r=[S[hh]], w=[Sbb])
                            psKb = nps()
                            mm(psKb[:, 0:128], Kd[64:128, cs], Vi[64:128, cs], True, True, r=[Kd, Vi], w=[psKb])
                            k.op("dve", lambda hh=hh, psKb=psKb, dec=dec: nc.vector.scalar_tensor_tensor(
                                out=S[hh][:, :], in0=S[hh][:, :], scalar=dec[:, 4 + hh:5 + hh],
                                in1=psKb[:, 0:128], op0=ALU.mult, op1=ALU.add), r=[S[hh], dec, psKb], w=[S[hh]])
                        if _CUT < 5:
                            continue
                        psO = nps()
                        mm(psO[0:P, 0:128], Am[0:P, 0:P], Vi[0:P, cs], True, False, r=[Am, Vi], w=[psO])
                        mm(psO[0:P, 0:128], qa[:, 0:P], Sa[:, :], False, not two, r=[qa, Sa], w=[psO])
                        if two:
                            mm(psO[0:P, 0:128], qb[:, 0:P], Sbb[:, :], False, True, r=[qb, Sbb], w=[psO])
                        if _CUT < 6:
                            continue
                        junk = f32t.get()
                        ss = small.get()
                        osb = f32t.get()
                        vcopy("act", osb[0:P, 0:128], psO[0:P, 0:128], r=[psO], w=[osb])
                        act(junk[0:P, 0:128], osb[0:P, 0:128], AF.Square, r=[osb], w=[junk, ss], accum_out=ss[0:P, 0:1])
                        k.op("dve", lambda ss=ss, P=P: nc.vector.tensor_scalar(
                            out=ss[0:P, 1:2], in0=ss[0:P, 0:1], scalar1=1.0 / 128, scalar2=RMS_EPS,
                            op0=ALU.mult, op1=ALU.add), r=[ss], w=[ss])
                        act(ss[0:P, 3:4], ss[0:P, 1:2], AF.Sqrt, r=[ss], w=[ss])
                        k.op("dve", lambda ss=ss, P=P: nc.vector.reciprocal(out=ss[0:P, 2:3], in_=ss[0:P, 3:4]),
                             r=[ss], w=[ss])
                        orec = smallb.get()
                        k.op("dve", lambda ss=ss, P=P, orec=orec, osb=osb, so=so, cs=cs: nc.vector.scalar_tensor_tensor(
                            out=orec[0:P, :], in0=osb[0:P, 0:128], scalar=ss[0:P, 2:3], in1=so[0:P, cs],
                            op0=ALU.mult, op1=ALU.mult), r=[ss, osb, so], w=[orec])
                        pT2 = npb()
                        tr(pT2[:, 0:P], orec[0:P, :], idb[0:P, 0:P], r=[orec, idb], w=[pT2])
                        vcopy("act", o_recT[:, h, t0:t0 + P], pT2[:, 0:P], r=[pT2], w=[o_recT.sub((h, ti))])
                for hh in range(4):
                    k.dma("sp", o_recs[4 * g + hh, :, :], S[hh][:, :], r=[S[hh]], is_out=True)

        k.barrier()
        if stage <= 1:
            k.finish()
            return nc
        SCALE = 128.0 ** -0.5
        with ExitStack() as pa:
            caus = SB(pa, [128, 2048], BF16, "caus")
            k.dma("pool", caus[:, :], CD["c_caus"].ap(), w=[caus])
            EE = SB(pa, [18, 2048], BF16, "EE")
            k.dma("pool", EE[:, :], CD["c_EE"].ap(), w=[EE])
            alb = cload(pa, "c_alb")
            gtab = cload(pa, "c_gtab")
            owt = cload(pa, "c_own")
            QsT = SB(pa, [128, 8, 4], F32, "QsT")
            KnT = SB(pa, [128, 8, 4], F32, "KnT")
            Vn = SB(pa, [4, 1024], F32, "Vn")
            with ExitStack() as pg_:
                xTo = SB(pg_, [128, 16, NTOK], BF16, "xTo2")
                xsrc = xT_own.rearrange("(dc p) t -> p dc t", p=128)
                for h in range(4):
                    k.dma("pool", xTo[:, 4 * h:4 * h + 4, :], xsrc[:, 4 * h:4 * h + 4, :], w=[xTo])
                wq2 = SB(pg_, [128, 16, 256], BF16, "wq2")
                wk2 = SB(pg_, [128, 16, 256], BF16, "wk2")
                wv2 = SB(pg_, [128, 16, 256], BF16, "wv2")
                KT = SB(pg_, [128, 2, 4096 + NS], BF16, "KT")
                V = SB(pg_, [128, 33, 256], BF16, "Vt")
                QT = SB(pg_, [128, 2, NTOK], BF16, "QT")
                RH = [[SB(pg_, [18, 512], BF16, "RH%d%d" % (a_, b_)) for b_ in range(2)] for a_ in range(2)]
                xtl = Rot(pg_, 3, [128, 16, 128], BF16, "axt")
                f32a = Rot(pg_, 4, [128, 256], F32, "af")
                bfa = Rot(pg_, 4, [128, 256], BF16, "ab")
                PTs = Rot(pg_, 3, [128, 512], BF16, "PT")
                gsm = Rot(pg_, 12, [128, 16], F32, "gsm")
                gmb = Rot(pg_, 4, [128, 16], BF16, "gmb")
                ksb = SB(pg_, [128, 2, 16], BF16, "ksb")
                rDt = SB(pg_, [128, 512], F32, "rD")
                for g2 in range(4):
                    set_rot([0, 1, 2, 3])
                    psO, psD = PS[4], PS[5]
                    load_w16(wq2, w_in[:, OFF_QA + g2 * 256: OFF_QA + (g2 + 1) * 256], 256)
                    load_w16(wk2, w_in[:, OFF_KA + g2 * 256: OFF_KA + (g2 + 1) * 256], 256)
                    load_w16(wv2, w_in[:, OFF_VA + g2 * 256: OFF_VA + (g2 + 1) * 256], 256)
                    for hh in range(2):
                        for qg in range(2):
                            h = 2 * g2 + hh
                            k.dma("pool", RH[hh][qg][16:18, :], CD["c_qal"].ap()[:, h * 512:(h + 1) * 512], w=[RH[hh][qg]])
                    for ti, (t0, P) in enumerate(tiles):
                        psq = proj(xTo, t0, P, wq2, 256)
                        psk = proj(xTo, t0, P, wk2, 256)
                        psv = proj(xTo, t0, P, wv2, 256)
                        qb = bfa.get()
                        act(qb[0:P, :], psq[0:P, 0:256], AF.Copy, r=[psq], w=[qb], scale=SCALE)
                        kf = f32a.get()
                        vcopy("act", kf[0:P, :], psk[0:P, 0:256], r=[psk], w=[kf])
                        vf = f32a.get()
                        vcopy("dve", vf[0:P, :], psv[0:P, 0:256], r=[psv], w=[vf])
                        k.dma("sp", o_kv[t0:t0 + P, g2 * 256:(g2 + 1) * 256], kf[0:P, :], r=[kf], is_out=True)
                        k.dma("sp", o_kv[t0:t0 + P, 1024 + g2 * 256:1024 + (g2 + 1) * 256], vf[0:P, :], r=[vf], is_out=True)
                        kb = bfa.get()
                        vcopy("dve", kb[0:P, :], kf[0:P, :], r=[kf], w=[kb])
                        vcopy("dve", V[0:P, 24 + ti, :], vf[0:P, :], r=[vf], w=[V.sub(24 + ti)])
                        if P != 128:
                            vcopy("act", Vn[0:P, g2 * 256:(g2 + 1) * 256], vf[0:P, :], r=[vf], w=[Vn])
                        for hh in range(2):
                            h = 2 * g2 + hh
                            cs = slice(hh * 128, (hh + 1) * 128)
                            pT = npb()
                            tr(pT[:, 0:P], qb[0:P, cs], idb[0:P, 0:P], r=[qb, idb], w=[pT])
                            tr(pT[:, 128:128 + P], kb[0:P, cs], idb[0:P, 0:P], r=[kb, idb], w=[pT])
                            ce = "act" if hh == 0 else "dve"
                            vcopy(ce, QT[:, hh, t0:t0 + P], pT[:, 0:P], r=[pT], w=[QT.sub((hh, ti))])
                            vcopy(ce, KT[:, hh, NPRE + t0:NPRE + t0 + P], pT[:, 128:128 + P], r=[pT], w=[KT.sub((hh, 24 + ti))])
                            if P != 128:
                                vcopy(ce, QsT[:, h, :], pT[:, 0:P], r=[pT], w=[QsT])
                                vcopy(ce, KnT[:, h, :], pT[:, 128:128 + P], r=[pT], w=[KnT])
                    for j in range(NT_PRE):
                        xt = xtl.get()
                        xs = xT_pre[:, j * 128:(j + 1) * 128].rearrange("(dc p) t -> p dc t", p=128)
                        k.dma("pool", xt[:, :, :], xs, w=[xt])
                        psk = proj(xt, 0, 128, wk2, 256)
                        psv = proj(xt, 0, 128, wv2, 256)
                        kb = bfa.get()
                        vcopy("act", kb[:, :], psk[:, 0:256], r=[psk], w=[kb])
                        vcopy("dve", V[:, j, :], psv[:, 0:256], r=[psv], w=[V.sub(j)])
                        pT = npb()
                        tr(pT[:, 0:128], kb[:, 0:128], idb[:, :], r=[kb, idb], w=[pT])
                        tr(pT[:, 128:256], kb[:, 128:256], idb[:, :], r=[kb, idb], w=[pT])
                        ce = "act" if j % 2 == 0 else "dve"
                        vcopy(ce, KT[:, 0, j * 128:(j + 1) * 128], pT[:, 0:128], r=[pT], w=[KT.sub((0, j))])
                        vcopy(ce, KT[:, 1, j * 128:(j + 1) * 128], pT[:, 128:256], r=[pT], w=[KT.sub((1, j))])
                    KTall = [[KT.sub((hh, j)) for j in range(33)] for hh in range(2)]
                    Vall = [V.sub(j) for j in range(33)]
                    QTall = [[QT.sub((hh, i)) for i in range(9)] for hh in range(2)]
                    for hh in range(2):
                        ks = gsm.get()
                        k.op("dve", lambda hh=hh, ks=ks: nc.vector.tensor_reduce(
                            out=ks[:, 0:16], in_=KT[:, hh, 0:4096].rearrange("p (n s) -> p n s", s=256),
                            axis=AX.X, op=ALU.add), r=KTall[hh], w=[ks])
                        vcopy("dve", ksb[:, hh, :], ks[:, 0:16], r=[ks], w=[ksb])
                    for hh in range(2):
                        for i in range(NT_OWN):
                            psg = nps()
                            mm(psg[:, 0:16], QT[:, hh, i * 128:(i + 1) * 128], ksb[:, hh, :], True, True,
                               r=[QTall[hh][i], ksb], w=[psg])
                            g1 = gsm.get()
                            tt(g1[:, :], psg[:, 0:16], gtab[:, i * 16:(i + 1) * 16], ALU.add, r=[psg, gtab], w=[g1])
                            mx = gsm.get()
                            k.op("dve", lambda mx=mx, g1=g1: nc.vector.max(out=mx[:, 0:8], in_=g1[:, :]), r=[g1], w=[mx])
                            sel = gsm.get()
                            k.op("dve", lambda sel=sel, g1=g1, mx=mx: nc.vector.tensor_scalar(
                                out=sel[:, :], in0=g1[:, :], scalar1=mx[:, 2:3], scalar2=None, op0=ALU.is_ge),
                                r=[g1, mx], w=[sel])
                            val = gsm.get()
                            k.op("dve", lambda val=val, g1=g1: nc.vector.tensor_scalar(
                                out=val[:, :], in0=g1[:, :], scalar1=-1.0e29, scalar2=None, op0=ALU.is_gt),
                                r=[g1], w=[val])
                            tt(sel[:, :], sel[:, :], val[:, :], ALU.mult, r=[sel, val], w=[sel])
                            tt(sel[:, :], sel[:, :], owt[:, i * 16:(i + 1) * 16], ALU.max, r=[sel, owt], w=[sel])
                            mb = gmb.get()
                            k.op("dve", lambda mb=mb, sel=sel: nc.vector.tensor_scalar(
                                out=mb[:, :], in0=sel[:, :], scalar1=-1.0, scalar2=1.0e30, op0=ALU.add, op1=ALU.mult),
                                r=[sel], w=[mb])
                            pT = npb()
                            tr(pT[0:16, 0:128], mb[:, 0:16], idb[:, :], r=[mb, idb], w=[pT])
                            rh = RH[hh][i // 4]
                            vcopy("act", rh[0:16, (i % 4) * 128:(i % 4 + 1) * 128], pT[0:16, 0:128], r=[pT], w=[rh])
                    for hh in range(2):
                        h = 2 * g2 + hh
                        for qg in range(2):
                            nkt = 24 + 4 * qg + 4
                            qsl = slice(qg * 512, (qg + 1) * 512)
                            qdeps = QTall[hh][4 * qg:4 * qg + 4]
                            rh = RH[hh][qg]
                            for kt in range(nkt):
                                m = kt - (24 + 4 * qg)
                                psS = nps()
                                mm(psS[:, :], KT[:, hh, kt * 128:(kt + 1) * 128], QT[:, hh, qsl], True, False,
                                   r=[KTall[hh][kt]] + qdeps, w=[psS])
                                mm(psS[:, :], EE[0:18, (kt // 2) * 128:(kt // 2 + 1) * 128], rh[0:18, :], False, m < 0,
                                   r=[EE, rh], w=[psS])
                                if m >= 0:
                                    mm(psS[:, :], idb[:, :], caus[:, m * 512:(m + 1) * 512], False, True,
                                       r=[idb, caus], w=[psS])
                                PT = PTs.get()
                                col = h * 32 + (m + 28)
                                act(PT[:, :], psS[:, :], AF.Exp, r=[psS, alb], w=[PT], bias=alb[:, col:col + 1])
                                mm(psO[:, :], V[:, kt, hh * 128:(hh + 1) * 128], PT[:, :], kt == 0, kt == nkt - 1,
                                   r=[Vall[kt], PT], w=[psO])
                                mm(psD[:, :], onesb[:, :], PT[:, :], kt == 0, kt == nkt - 1, r=[onesb, PT], w=[psD])
                            k.op("dve", lambda: nc.vector.reciprocal(out=rDt[:, :], in_=psD[:, :]), r=[psD], w=[rDt])
                            tt(o_attnT[:, h, qsl], psO[:, :], rDt[:, :], ALU.mult, r=[psO, rDt], w=[o_attnT.sub((h, qg))])

            k.barrier()
            if stage <= 2:
                k.finish()
                return nc

            with ExitStack() as psm:
                set_rot([0, 1, 2, 3])
                psKS, psOs = PS[4], PS[5]
                iota = cload(psm, "c_iota")
                bias_s = cload(psm, "c_bias_s")
                caus_s = cload(psm, "c_caus_s")
                oneq = cload(psm, "c_oneq")
                ptb = SB(psm, [128, 128], I32, "ptb")
                k.dma("sp", ptb[:, :], pt.ap().partition_broadcast(128), w=[ptb])
                ptf = SB(psm, [128, 128], F32, "ptf")
                vcopy("dve", ptf[:, :], ptb[:, :], r=[ptb], w=[ptf])
                k.op("dve", lambda: nc.vector.tensor_scalar(out=ptf[:, :], in0=ptf[:, :], scalar1=128.0,
                                                            scalar2=iota[:, 0:1], op0=ALU.mult, op1=ALU.add),
                     r=[ptf, iota], w=[ptf])
                idx = SB(psm, [128, 128], I32, "idx")
                vcopy("dve", idx[:, :], ptf[:, :], r=[ptf], w=[idx])
                Sall = SB(psm, [128, 8, 4, 2, 64], F32, "Sall")
                pages = Rot(psm, 3, [128, 1024], F32, "pg")
                kTs = Rot(psm, 2, [128, 1024], F32, "kTs")
                for pg in range(128):
                    n, a = divmod(pg, 2)
                    kp = pages.get()
                    k.gather(kp[:, :], ck.ap(), idx[:, pg:pg + 1], r=[idx], w=[kp])
                    for h in range(8):
                        mm(psKS[:, h * 64 + n:h * 64 + n + 1], kp[:, h * 128:(h + 1) * 128], onesf[:, 0:1],
                           pg == 0 and h == 0, pg == 127 and h == 7, r=[kp, onesf], w=[psKS])
                    kT = kTs.get()
                    for half in range(2):
                        pTf = nps()
                        for hq in range(4):
                            h = half * 4 + hq
                            mm(pTf[:, hq * 128:(hq + 1) * 128], kp[:, h * 128:(h + 1) * 128], idf[:, :], True, True,
                               r=[kp, idf], w=[pTf])
                        vcopy("act" if half == 0 else "dve", kT[:, half * 512:(half + 1) * 512], pTf[:, :],
                              r=[pTf], w=[kT.sub(half)])
                    psSc = nps()
                    for h in range(8):
                        mm(psSc[:, h * 4:(h + 1) * 4], kT[:, h * 128:(h + 1) * 128], QsT[:, h, :], True, True,
                           r=[kT.sub(h // 4), QsT], w=[psSc])
                    vcopy("act", Sall[:, :, :, a, n], psSc[:, 0:32].rearrange("p (h q) -> p h q", q=4),
                          r=[psSc], w=[Sall])
                ksT = SB(psm, [128, 512], F32, "ksT")
                vcopy("dve", ksT[:, :], psKS[:, :], r=[psKS], w=[ksT])
                psGt = nps()
                for h in range(8):
                    mm(psGt[0:4, h * 64:(h + 1) * 64], QsT[:, h, :], ksT[:, h * 64:(h + 1) * 64], True, True,
                       r=[QsT, ksT], w=[psGt])
                gsb = SB(psm, [4, 512], F32, "gsb")
                vcopy("dve", gsb[:, :], psGt[0:4, :], r=[psGt], w=[gsb])
                mx8 = SB(psm, [4, 64], F32, "mx8")
                mbs = SB(psm, [4, 512], F32, "mbs")
                for h in range(8):
                    hs = slice(h * 64, (h + 1) * 64)
                    k.op("dve", lambda h=h, hs=hs: nc.vector.max(out=mx8[:, h * 8:(h + 1) * 8], in_=gsb[:, hs]),
                         r=[gsb], w=[mx8])
                    k.op("dve", lambda h=h, hs=hs: nc.vector.tensor_scalar(
                        out=mbs[:, hs], in0=gsb[:, hs], scalar1=mx8[:, h * 8 + 2:h * 8 + 3], scalar2=None,
                        op0=ALU.is_ge), r=[gsb, mx8], w=[mbs])
                k.op("dve", lambda: nc.vector.tensor_scalar(out=mbs[:, :], in0=mbs[:, :], scalar1=-1.0, scalar2=1.0e30,
                                                            op0=ALU.add, op1=ALU.mult), r=[mbs], w=[mbs])
                for q in range(4):
                    psB = nps()
                    mm(psB[:, :], oneq[0:4, q * 128:(q + 1) * 128], mbs[0:4, :], True, True, r=[oneq, mbs], w=[psB])
                    for a in range(2):
                        tt(Sall[:, :, q, a, :], Sall[:, :, q, a, :], psB[:, :].rearrange("p (h n) -> p h n", n=64),
                           ALU.add, r=[Sall, psB], w=[Sall])
                    tt(Sall[:, :, q, :, :], Sall[:, :, q, :, :],
                       bias_s[:, :].rearrange("p (h a n) -> p h a n", a=2, n=64), ALU.add, r=[Sall, bias_s], w=[Sall])
                for h in range(8):
                    act(Sall[:, h, :, :, :], Sall[:, h, :, :, :], AF.Exp, r=[Sall], w=[Sall])
                psOw = nps()
                for h in range(8):
                    mm(psOw[0:4, h * 4:(h + 1) * 4], KnT[:, h, :], QsT[:, h, :], True, True, r=[KnT, QsT], w=[psOw])
                pown = SB(psm, [4, 32], F32, "pown")
                tt(pown[:, :], psOw[0:4, 0:32], caus_s[:, :], ALU.add, r=[psOw, caus_s], w=[pown])
                act(pown[:, :], pown[:, :], AF.Exp, r=[pown], w=[pown])
                psDs = nps()
                for pg in range(128):
                    n, a = divmod(pg, 2)
                    vp = pages.get()
                    k.gather(vp[:, :], cv.ap(), idx[:, pg:pg + 1], r=[idx], w=[vp])
                    for h in range(8):
                        mm(psOs[:, h * 4:(h + 1) * 4], vp[:, h * 128:(h + 1) * 128], Sall[:, h, :, a, n],
                           pg == 0 and h == 0, False, r=[vp, Sall], w=[psOs])
                    mm(psDs[:, 0:32], onesf[:, :], Sall[:, :, :, a, n], pg == 0, False, r=[onesf, Sall], w=[psDs])
                for h in range(8):
                    mm(psOs[:, h * 4:(h + 1) * 4], Vn[0:4, h * 128:(h + 1) * 128], pown[0:4, h * 4:(h + 1) * 4],
                       False, h == 7, r=[Vn, pown], w=[psOs])
                mm(psDs[:, 0:32], onesf[0:4, :], pown[0:4, :], False, True, r=[onesf, pown], w=[psDs])
                rDs = SB(psm, [128, 32], F32, "rDs")
                k.op("dve", lambda: nc.vector.reciprocal(out=rDs[:, :], in_=psDs[:, 0:32]), r=[psDs], w=[rDs])
                tt(o_attnT[:, :, NOWN:NOWN + NS], psOs[:, 0:32].rearrange("p (h q) -> p h q", q=4),
                   rDs[:, :].rearrange("p (h q) -> p h q", q=4), ALU.mult, r=[psOs, rDs], w=[o_attnT.sub("s")])

        k.barrier()
        if stage <= 3:
            k.finish()
            return nc
        with ExitStack() as pm:
            set_rot([0, 1, 2, 3, 4, 5])
            mergedT = SB(pm, [128, 16, NTOK], BF16, "mergedT")
            with ExitStack() as pm1:
                xTo = SB(pm1, [128, 16, NTOK], BF16, "xTo3")
                xsrc = xT_own.rearrange("(dc p) t -> p dc t", p=128)
                for h in range(4):
                    k.dma("pool", xTo[:, 4 * h:4 * h + 4, :], xsrc[:, 4 * h:4 * h + 4, :], w=[xTo])
                wpa = SB(pm1, [128, 8, 512], BF16, "wpa")
                wpb = SB(pm1, [128, 8, 512], BF16, "wpb")
                wga = SB(pm1, [128, 16, 512], BF16, "wga")
                wgb = SB(pm1, [128, 16, 512], BF16, "wgb")
                mf = Rot(pm1, 6, [128, 512], F32, "mf")
                mgb = Rot(pm1, 2, [128, 512], BF16, "mgb")
                for dmb in range(4):
                    dsl = slice(dmb * 512, (dmb + 1) * 512)
                    k.dma("pool", wpa[:, :, :], w_pa[:, dsl].rearrange("(h p) n -> p h n", p=128), w=[wpa])
                    k.dma("pool", wpb[:, :, :], w_pb[:, dsl].rearrange("(h p) n -> p h n", p=128), w=[wpb])
                    load_w16(wga, w_in[:, OFF_GA + dmb * 512:OFF_GA + (dmb + 1) * 512], 512)
                    load_w16(wgb, w_in[:, OFF_GB + dmb * 512:OFF_GB + (dmb + 1) * 512], 512)
                    for ti, (t0, P) in enumerate(tiles):
                        psA = nps()
                        for h in range(8):
                            mm(psA[0:P, :], o_attnT[:, h, t0:t0 + P], wpa[:, h, :], h == 0, h == 7,
                               r=[o_attnT.sub((h, 0)), o_attnT.sub((h, 1)), o_attnT.sub("s"), wpa], w=[psA])
                        psB = nps()
                        for h in range(8):
                            mm(psB[0:P, :], o_recT[:, h, t0:t0 + P], wpb[:, h, :], h == 0, h == 7,
                               r=[o_recT.sub((h, ti)), wpb], w=[psB])
                        psga = proj(xTo, t0, P, wga, 512)
                        psgb = proj(xTo, t0, P, wgb, 512)
                        sga = mf.get()
                        act(sga[0:P, :], psga[0:P, :], AF.Sigmoid, r=[psga], w=[sga])
                        sgb = mf.get()
                        act(sgb[0:P, :], psgb[0:P, :], AF.Sigmoid, r=[psgb], w=[sgb])
                        m1 = mf.get()
                        tt(m1[0:P, :], sga[0:P, :], psA[0:P, :], ALU.mult, r=[sga, psA], w=[m1])
                        tt(sgb[0:P, :], sgb[0:P, :], psB[0:P, :], ALU.mult, r=[sgb, psB], w=[sgb])
                        mg = mgb.get()
                        tt(mg[0:P, :], m1[0:P, :], sgb[0:P, :], ALU.add, r=[m1, sgb], w=[mg])
                        pT = npb()
                        for j in range(4):
                            tr(pT[:, j * 128:j * 128 + P], mg[0:P, j * 128:(j + 1) * 128], idb[0:P, 0:P], r=[mg, idb], w=[pT])
                        vcopy("act", mergedT[:, dmb * 4:(dmb + 1) * 4, t0:t0 + P],
                              pT[:, :].rearrange("p (j t) -> p j t", t=128)[:, :, 0:P], r=[pT], w=[mergedT.sub((dmb, ti))])
            k.barrier()
            MT = [[mergedT.sub((d_, t_)) for d_ in range(4)] for t_ in range(9)]
            if stage <= 4:
                k.finish()
                return nc
            buf = SB(pm, [128, 9, 2048], F32, "buf")
            for ti, (t0, P) in enumerate(tiles):
                k.dma("sp", buf[0:P, ti, :], x_own[t0:t0 + P, :], w=[buf.sub(ti)])
            lnsm = Rot(pm, 6, [128, 32], F32, "lnsm")

            def layer_norm_tiles(lnp, after):
                with ExitStack() as pl:
                    k.barrier()
                    Gt = SB(pl, [128, 2048], F32)
                    Bt = SB(pl, [128, 2048], F32)
                    k.dma("sp", Gt[:, :], lnp[0:1, :].partition_broadcast(128), w=[Gt])
                    k.dma("sp", Bt[:, :], lnp[1:2, :].partition_broadcast(128), w=[Bt])
                    for ti, (t0, P) in enumerate(tiles):
                        bt = buf.sub(ti)
                        st_ = lnsm.get()
                        for c_ in range(4):
                            k.op("dve", lambda c_=c_, st_=st_, ti=ti, P=P: nc.vector.bn_stats(
                                out=st_[0:P, c_ * 6:(c_ + 1) * 6], in_=buf[0:P, ti, c_ * 512:(c_ + 1) * 512]),
                                r=[bt], w=[st_])
                        mv = lnsm.get()
                        k.op("dve", lambda st_=st_, mv=mv, P=P: nc.vector.bn_aggr(
                            out=mv[0:P, 0:2], in_=st_[0:P, 0:24].rearrange("p (c s) -> p c s", s=6)), r=[st_], w=[mv])
                        k.op("dve", lambda mv=mv, P=P: nc.vector.tensor_scalar(
                            out=mv[0:P, 2:3], in0=mv[0:P, 1:2], scalar1=LN_EPS, scalar2=None, op0=ALU.add),
                            r=[mv], w=[mv])
                        act(mv[0:P, 3:4], mv[0:P, 2:3], AF.Sqrt, r=[mv], w=[mv])
                        k.op("dve", lambda mv=mv, P=P: nc.vector.reciprocal(out=mv[0:P, 4:5], in_=mv[0:P, 3:4]),
                             r=[mv], w=[mv])
                        k.op("dve", lambda mv=mv, P=P, ti=ti: nc.vector.tensor_scalar(
                            out=buf[0:P, ti, :], in0=buf[0:P, ti, :], scalar1=mv[0:P, 0:1], scalar2=mv[0:P, 4:5],
                            op0=ALU.subtract, op1=ALU.mult), r=[mv, bt], w=[bt])
                        tt(buf[0:P, ti, :], buf[0:P, ti, :], Gt[0:P, :], ALU.mult, r=[bt, Gt], w=[bt])
                        tt(buf[0:P, ti, :], buf[0:P, ti, :], Bt[0:P, :], ALU.add, r=[bt, Bt], w=[bt])
                        after(ti, t0, P)

            with ExitStack() as pm2:
                wos = Rot(pm2, 2, [128, 16, 512], BF16, "wo")
                for dmb in range(4):
                    wo_ = wos.get()
                    load_w16(wo_, w_out[:, dmb * 512:(dmb + 1) * 512], 512)
                    for ti, (t0, P) in enumerate(tiles):
                        ps = nps()
                        for dc in range(16):
                            mm(ps[0:P, :], mergedT[:, dc, t0:t0 + P], wo_[:, dc, :], dc == 0, dc == 15,
                               r=[MT[ti][dc // 4], wo_], w=[ps])
                        k.op("dve", lambda ps=ps, ti=ti, P=P, dmb=dmb: nc.vector.scalar_tensor_tensor(
                            out=buf[0:P, ti, dmb * 512:(dmb + 1) * 512], in0=buf[0:P, ti, dmb * 512:(dmb + 1) * 512],
                            scalar=ALPHA, in1=ps[0:P, :], op0=ALU.mult, op1=ALU.add), r=[ps, buf.sub(ti)], w=[buf.sub(ti)])
            k.barrier()
            x1T = mergedT
            cgate = SB(pm, [128, 9, 32], F32, "cgate")
            wrt = SB(pm, [128, 16, 36], BF16, "wrt")
            k.dma("pool", wrt[:, :, :], w_rt.rearrange("(dc p) n -> p dc n", p=128), w=[wrt])
            brt = SB(pm, [128, 36], F32, "brt")
            k.dma("sp", brt[:, :], b_rt[0:1, :].partition_broadcast(128), w=[brt])
            x1b = Rot(pm, 2, [128, 2048], BF16, "x1b")
            rsm = Rot(pm, 16, [128, 36], F32, "rsm")

            def after_ln1(ti, t0, P):
                bt = buf.sub(ti)
                xb = x1b.get()
                vcopy("act", xb[0:P, :], buf[0:P, ti, :], r=[bt], w=[xb])
                for q4 in range(4):
                    pT = npb()
                    for j in range(4):
                        dc = q4 * 4 + j
                        tr(pT[:, j * 128:j * 128 + P], xb[0:P, dc * 128:(dc + 1) * 128], idb[0:P, 0:P], r=[xb, idb], w=[pT])
                    vcopy("act" if q4 % 2 == 0 else "dve", x1T[:, q4 * 4:(q4 + 1) * 4, t0:t0 + P],
                          pT[:, :].rearrange("p (j t) -> p j t", t=128)[:, :, 0:P], r=[pT], w=[x1T.sub((q4, ti))])
                k.op("dve", lambda: nc.vector.tensor_scalar(out=buf[0:P, ti, :], in0=buf[0:P, ti, :], scalar1=ALPHA,
                                                            scalar2=None, op0=ALU.mult), r=[bt], w=[bt])
                psr = nps()
                for dc in range(16):
                    mm(psr[0:P, 0:36], x1T[:, dc, t0:t0 + P], wrt[:, dc, :], dc == 0, dc == 15,
                       r=[x1T.sub((dc // 4, ti)), wrt], w=[psr])
                lg = rsm.get()
                tt(lg[0:P, :], psr[0:P, 0:36], brt[0:P, :], ALU.add, r=[psr, brt], w=[lg])
                g8 = rsm.get()
                k.op("dve", lambda: nc.vector.memset(g8[0:P, 0:8], NEG), w=[g8])
                vcopy("dve", g8[0:P, 0:4], lg[0:P, 0:4], r=[lg], w=[g8])
                mx = rsm.get()
                k.op("dve", lambda: nc.vector.max(out=mx[0:P, 0:8], in_=g8[0:P, 0:8]), r=[g8], w=[mx])
                oh = rsm.get()
                k.op("dve", lambda: nc.vector.tensor_scalar(out=oh[0:P, 0:4], in0=lg[0:P, 0:4], scalar1=mx[0:P, 0:1],
                                                            scalar2=None, op0=ALU.is_ge), r=[lg, mx], w=[oh])
                k.op("dve", lambda: nc.vector.tensor_scalar(out=mx[0:P, 8:9], in0=mx[0:P, 0:1], scalar1=-1.0,
                                                            scalar2=None, op0=ALU.mult), r=[mx], w=[mx])
                eg = rsm.get()
                act(eg[0:P, 0:4], lg[0:P, 0:4], AF.Exp, r=[lg, mx], w=[eg], bias=mx[0:P, 8:9], accum_out=eg[0:P, 8:9])
                k.op("dve", lambda: nc.vector.reciprocal(out=eg[0:P, 9:10], in_=eg[0:P, 8:9]), r=[eg], w=[eg])
                el = rsm.get()
                k.op("dve", lambda: nc.vector.tensor_scalar(out=el[0:P, 0:8], in0=lg[0:P, 4:12], scalar1=oh[0:P, 0:1],
                                                            scalar2=None, op0=ALU.mult), r=[lg, oh], w=[el])
                for g_ in range(1, 4):
                    k.op("dve", lambda g_=g_: nc.vector.scalar_tensor_tensor(
                        out=el[0:P, 0:8], in0=lg[0:P, 4 + 8 * g_:12 + 8 * g_], scalar=oh[0:P, g_:g_ + 1],
                        in1=el[0:P, 0:8], op0=ALU.mult, op1=ALU.add), r=[lg, oh, el], w=[el])
                m2 = rsm.get()
                k.op("dve", lambda: nc.vector.max(out=m2[0:P, 0:8], in_=el[0:P, 0:8]), r=[el], w=[m2])
                se = rsm.get()
                k.op("dve", lambda: nc.vector.tensor_scalar(out=se[0:P, 0:8], in0=el[0:P, 0:8], scalar1=m2[0:P, 1:2],
                                                            scalar2=None, op0=ALU.is_ge), r=[el, m2], w=[se])
                k.op("dve", lambda: nc.vector.tensor_scalar(out=m2[0:P, 8:9], in0=m2[0:P, 0:1], scalar1=-1.0,
                                                            scalar2=None, op0=ALU.mult), r=[m2], w=[m2])
                ex = rsm.get()
                act(ex[0:P, 0:8], el[0:P, 0:8], AF.Exp, r=[el, m2], w=[ex], bias=m2[0:P, 8:9])
                tt(ex[0:P, 0:8], ex[0:P, 0:8], se[0:P, 0:8], ALU.mult, r=[ex, se], w=[ex])
                k.op("dve", lambda: nc.vector.tensor_reduce(out=ex[0:P, 8:9], in_=ex[0:P, 0:8], axis=AX.X, op=ALU.add),
                     r=[ex], w=[ex])
                k.op("dve", lambda: nc.vector.reciprocal(out=ex[0:P, 9:10], in_=ex[0:P, 8:9]), r=[ex], w=[ex])
                tt(ex[0:P, 9:10], ex[0:P, 9:10], eg[0:P, 9:10], ALU.mult, r=[ex, eg], w=[ex])
                k.op("dve", lambda: nc.vector.tensor_scalar(out=ex[0:P, 0:8], in0=ex[0:P, 0:8], scalar1=ex[0:P, 9:10],
                                                            scalar2=None, op0=ALU.mult), r=[ex], w=[ex])
                for g_ in range(4):
                    k.op("dve", lambda g_=g_: nc.vector.tensor_scalar(
                        out=cgate[0:P, ti, 8 * g_:8 * g_ + 8], in0=ex[0:P, 0:8], scalar1=oh[0:P, g_:g_ + 1],
                        scalar2=None, op0=ALU.mult), r=[ex, oh], w=[cgate])

            layer_norm_tiles(ln1, after_ln1)
            k.barrier()
            if stage <= 5:
                k.finish()
                return nc
            X1 = [[x1T.sub((q4, ti)) for q4 in range(4)] for ti in range(9)]
            with ExitStack() as pm3:
                class _Alias:
                    def __init__(self, src):
                        self.src = src
                        self.b = Buf()
                        for sb_ in src.subs.values():
                            self.b.r.extend(sb_.r)
                            if sb_.w is not None:
                                self.b.r.append(sb_.w)
                        self.v = src[:, :, :].rearrange("p h t -> p (h t)")

                    def __getitem__(self, key):
                        return self.v[key]

                class _WE:
                    def __init__(self):
                        self.ts = [_Alias(o_recT), _Alias(o_attnT), SB(pm3, [128, 8192], BF16, "wE2"), SB(pm3, [128, 8192], BF16, "wE3")]
                        self.i = 0

                    def get(self):
                        t = self.ts[self.i % 4]
                        self.i += 1
                        return t
                wE = _WE()
                hid = SB(pm3, [128, 4, NTOK], BF16, "hid")
                s1t = Rot(pm3, 3, [128, 512], F32, "s1t")
                groups = [(0, 343), (343, 343), (686, 342)]
                import os as _os2
                NE = int(_os2.environ.get("MOE_NE", "32"))
                for e in range(NE):
                    w1t = wE.get()
                    w3t = wE.get()
                    w2t = wE.get()
                    v1 = w1[e].rearrange("(dc p) f -> p dc f", p=128)
                    v3 = w3[e].rearrange("(dc p) f -> p dc f", p=128)
                    v2 = w2[e].rearrange("(fc p) d -> p fc d", p=128)
                    w1v = w1t[:, 0:8192].rearrange("p (dc f) -> p dc f", f=512)
                    w3v = w3t[:, 0:8192].rearrange("p (dc f) -> p dc f", f=512)
                    w2v = w2t[:, 0:8192].rearrange("p (fc d) -> p fc d", d=2048)
                    for hf in range(2):
                        k.dma("pool", w1v[:, 8 * hf:8 * hf + 8, :], v1[:, 8 * hf:8 * hf + 8, :], w=[w1t])
                    for hf in range(2):
                        k.dma("pool", w3v[:, 8 * hf:8 * hf + 8, :], v3[:, 8 * hf:8 * hf + 8, :], w=[w3t])
                    for hf in range(2):
                        k.dma("pool", w2v[:, 2 * hf:2 * hf + 2, :], v2[:, 2 * hf:2 * hf + 2, :], w=[w2t])
                    for gi, (c0, n) in enumerate(groups):
                        xdeps = [X1[ti][q4] for ti in range(9) for q4 in range(4) if (tiles[ti][0] < c0 + n and tiles[ti][0] + tiles[ti][1] > c0)]
                        for fc in range(4):
                            ps1 = nps()
                            for dc in range(16):
                                mm(ps1[:, 0:n], w1v[:, dc, fc * 128:(fc + 1) * 128], x1T[:, dc, c0:c0 + n], dc == 0, dc == 15,
                                   r=[w1t] + xdeps, w=[ps1])
                            ps3 = nps()
                            for dc in range(16):
                                mm(ps3[:, 0:n], w3v[:, dc, fc * 128:(fc + 1) * 128], x1T[:, dc, c0:c0 + n], dc == 0, dc == 15,
                                   r=[w3t] + xdeps, w=[ps3])
                            s1 = s1t.get()
                            act(s1[:, 0:n], ps1[:, 0:n], AF.Silu, r=[ps1], w=[s1])
                            tt(hid[:, fc, c0:c0 + n], s1[:, 0:n], ps3[:, 0:n], ALU.mult, r=[s1, ps3], w=[hid.sub((fc, gi))])
                    for ti, (t0, P) in enumerate(tiles):
                        gis = [gi_ for gi_, (c0_, n_) in enumerate(groups) if (t0 < c0_ + n_ and t0 + P > c0_)]
                        for dmb in range(4):
                            psy = nps()
                            for fc in range(4):
                                mm(psy[0:P, :], hid[:, fc, t0:t0 + P], w2v[:, fc, dmb * 512:(dmb + 1) * 512], fc == 0, fc == 3,
                                   r=[hid.sub((fc, gi_)) for gi_ in gis] + [w2t], w=[psy])
                            k.op("dve", lambda psy=psy, ti=ti, P=P, dmb=dmb, e=e: nc.vector.scalar_tensor_tensor(
                                out=buf[0:P, ti, dmb * 512:(dmb + 1) * 512], in0=psy[0:P, :], scalar=cgate[0:P, ti, e:e + 1],
                                in1=buf[0:P, ti, dmb * 512:(dmb + 1) * 512], op0=ALU.mult, op1=ALU.add),
                                r=[psy, cgate, buf.sub(ti)], w=[buf.sub(ti)])

            k.barrier()
            def after_ln2(ti, t0, P):
                k.dma("sp", o_y[t0:t0 + P, :], buf[0:P, ti, :], r=[buf.sub(ti)], is_out=True)

            layer_norm_tiles(ln2, after_ln2)
        k.finish()
    return nc


def _prep_inputs(inputs):
    f = lambda n: np.asarray(inputs[n], dtype=np.float32)
    x_prompt = f("x_prompt")
    x_sample = f("x_sample")
    shared = {
        "w_in": np.ascontiguousarray(f("w_in")[0]),
        "w_pa": np.ascontiguousarray(f("w_pa")[0]),
        "w_pb": np.ascontiguousarray(f("w_pb")[0]),
        "w_out": np.ascontiguousarray(f("w_out")[0]),
        "lbl": np.ascontiguousarray(f("lb_logits")),
        "rng": np.ascontiguousarray(f("rec_norm_g")),
        "ln1": np.ascontiguousarray(np.concatenate([f("ln1_g"), f("ln1_b")], 0)),
        "ln2": np.ascontiguousarray(np.concatenate([f("ln2_g"), f("ln2_b")], 0)),
        "w_rt": np.ascontiguousarray(np.concatenate(
            [f("w_group")[0], f("w_router")[0].transpose(1, 0, 2).reshape(D, 32)], 1)),
        "b_rt": np.ascontiguousarray(np.concatenate(
            [f("b_group")[0].reshape(1, 4), f("b_router")[0].reshape(1, 32)], 1)),
        "w1": np.ascontiguousarray(f("w1")[0]),
        "w3": np.ascontiguousarray(f("w3")[0]),
        "w2": np.ascontiguousarray(f("w2")[0]),
        "ck": np.ascontiguousarray(f("cache_k")[0]).reshape(163840, 1024),
        "cv": np.ascontiguousarray(f("cache_v")[0]).reshape(163840, 1024),
    }
    page_table = np.asarray(inputs["page_table"]).astype(np.int32)
    state_rec = f("state_rec")[0]
    maps = []
    for c in range(8):
        b, qt = divmod(c, 4)
        xp = x_prompt[b]
        pre = np.zeros((NPRE, D), np.float32)
        n_pre = qt * 1024
        if n_pre:
            pre[NPRE - n_pre:] = xp[:n_pre]
        own = np.concatenate([xp[qt * 1024:(qt + 1) * 1024], x_sample[c]], 0)
        m = dict(shared)
        m.update({
            "xT_pre": np.ascontiguousarray(pre.T),
            "xT_own": np.ascontiguousarray(own.T),
            "x_own": np.ascontiguousarray(own),
            "pt": np.ascontiguousarray(page_table[c].reshape(1, 128)),
            "st": np.ascontiguousarray(state_rec[c]),
        })
        m.update(_host_consts(qt))
        maps.append(m)
    return maps


def _run(inputs, stage=99):
    nc = build(stage)
    maps = _prep_inputs(inputs)
    used = set(nc._used_inputs.keys())
    maps = [{n: v for n, v in m.items() if n in used} for m in maps]
    res = run_bass_kernel_spmd(nc, maps, core_ids=list(range(8)))
    return res


def kernel(**inputs):
    res = _run(inputs)
    R = res.results
    y_prompt = np.zeros((2, 4096, D), np.float32)
    y_sample = np.zeros((8, 4, D), np.float32)
    k_prompt = np.zeros((1, 2, 4096, 8, 128), np.float32)
    v_prompt = np.zeros((1, 2, 4096, 8, 128), np.float32)
    rec_prompt = np.zeros((1, 2, 8, 128, 128), np.float32)
    k_sample = np.zeros((1, 8, 4, 8, 128), np.float32)
    v_sample = np.zeros((1, 8, 4, 8, 128), np.float32)
    rec_sample = np.zeros((1, 8, 8, 128, 128), np.float32)
    for c in range(8):
        b, qt = divmod(c, 4)
        sl = slice(qt * 1024, (qt + 1) * 1024)
        oy = np.asarray(R[c]["o_y"])
        okv = np.asarray(R[c]["o_kv"])
        y_prompt[b, sl] = oy[:1024]
        y_sample[c] = oy[1024:]
        k_prompt[0, b, sl] = okv[:1024, :1024].reshape(1024, 8, 128)
        v_prompt[0, b, sl] = okv[:1024, 1024:].reshape(1024, 8, 128)
        k_sample[0, c] = okv[1024:, :1024].reshape(4, 8, 128)
        v_sample[0, c] = okv[1024:, 1024:].reshape(4, 8, 128)
        rec_sample[0, c] = np.asarray(R[c]["o_recs"])
        if qt == 3:
            rec_prompt[0, b] = np.asarray(R[c]["o_recp"])
    return (y_prompt, y_sample, k_prompt, v_prompt, rec_prompt, k_sample, v_sample, rec_sample)
```
